# Optimizing a Trainium2 kernel written in Bass

```python
import math
import jax, jax.numpy as jnp
from jax import lax
import numpy as np

D_MODEL = 1024
BATCH = 4
SEQ = 8192
DEPTH = 1

CHUNK = 64
N_META = 16
META_PAD = (CHUNK - N_META % CHUNK) % CHUNK
RMS_EPS = 1e-6
GLA_HEADS = 4
GLA_DK = D_MODEL // 8
GLA_DV = D_MODEL // 4
GLA_GATE_RANK = 16
GLA_GATE_NORM = 16.0
GLA_QK = GLA_HEADS * GLA_DK
GLA_VW = GLA_HEADS * GLA_DV
SSD_D_INNER = 2 * D_MODEL
SSD_HEADDIM = 64
SSD_HEADS = SSD_D_INNER // SSD_HEADDIM
SSD_GROUPS = 4
SSD_HPG = SSD_HEADS // SSD_GROUPS
SSD_STATE = 128
SSD_CONV = 4
SSD_BC = SSD_GROUPS * SSD_STATE
SSD_XBC = SSD_D_INNER + 2 * SSD_BC
PEER_HEADS = 8
PEER_N_KEYS = 128
PEER_N_EXPERTS = PEER_N_KEYS * PEER_N_KEYS
PEER_KEY_DIM = 256
PEER_HALF = PEER_KEY_DIM // 2
PEER_TOPK = 16
PEER_BLOCK = 256
IN_SPLITS = (GLA_QK, GLA_QK, GLA_VW, GLA_VW, GLA_GATE_RANK, SSD_D_INNER, SSD_XBC, SSD_HEADS, D_MODEL, D_MODEL)
IN_WIDTH = GLA_QK * 2 + GLA_VW * 2 + GLA_GATE_RANK + SSD_D_INNER + SSD_XBC + SSD_HEADS + 2 * D_MODEL

kernel_name = 'hybrid_gla_ssd_peer_streaming_block'


def rmsnorm(x, w):
    xf = x.astype(jnp.float32)
    y = xf * lax.rsqrt(jnp.mean(xf * xf, axis=-1, keepdims=True) + RMS_EPS)
    return (y * w.astype(jnp.float32)).astype(x.dtype)


def split_cols(t, sizes):
    out, start = [], 0
    for s in sizes:
        out.append(t[..., start:start + s])
        start += s
    return out


def pad_front(t):
    return jnp.pad(t, [(0, 0), (META_PAD, 0)] + [(0, 0)] * (t.ndim - 2))


def to_chunks(t):
    b, lp = t.shape[:2]
    return jnp.moveaxis(t.reshape(b, lp // CHUNK, CHUNK, *t.shape[2:]), 1, 0)


def from_chunks(t):
    t = jnp.moveaxis(t, 0, 1)
    return t.reshape(t.shape[0], -1, *t.shape[3:])


def causal_dwconv(u, w, b):
    L = u.shape[1]
    up = jnp.pad(u, ((0, 0), (SSD_CONV - 1, 0), (0, 0)))
    return sum(up[:, i:i + L] * w[i] for i in range(SSD_CONV)) + b


def gla_chunk_step(S, inp):
    q, k, v, lg = inp
    G = jnp.cumsum(lg, axis=1)
    eG, eGn = jnp.exp(G), jnp.exp(-G)
    a_causal = jnp.einsum('bthk,bshk->bhts', q * eG, k * eGn)
    a_ahead = jnp.einsum('bthk,bshk->bhts', q * eGn, k * eG)
    tri = jnp.tril(jnp.ones((CHUNK, CHUNK), dtype=bool))
    att = jnp.where(tri, a_causal, a_ahead)
    o = jnp.einsum('bhts,bshv->bthv', att, v) + jnp.einsum('bthk,bhkv->bthv', q * eG, S)
    G_last = G[:, -1]
    S = S * jnp.exp(G_last)[..., None] + jnp.einsum('bshk,bshv->bhkv', k * jnp.exp(G_last[:, None] - G), v)
    return S, o


def gla_chunked(q, k, v, lg):
    S0 = jnp.zeros((q.shape[0], GLA_HEADS, GLA_DK, GLA_DV), jnp.float32)
    _, o = lax.scan(gla_chunk_step, S0, (to_chunks(q), to_chunks(k), to_chunks(v), to_chunks(lg)))
    return from_chunks(o)


def ssd_chunk_step(S, inp):
    xdt, a, Bc, Cc = inp
    acum = jnp.cumsum(a, axis=1)
    seg = jnp.exp(-jnp.abs(acum[:, :, None] - acum[:, None, :]))
    cb = jnp.einsum('btgn,bsgn->btsg', Cc, Bc)
    y = jnp.einsum('btsgr,btsg,bsgrp->btgrp', seg, cb, xdt)
    y = y + jnp.einsum('btgn,bgrpn,btgr->btgrp', Cc, S, jnp.exp(acum))
    to_end = jnp.exp(acum[:, -1:] - acum)
    S = S * jnp.exp(acum[:, -1])[..., None, None] + jnp.einsum('bsgn,bsgr,bsgrp->bgrpn', Bc, to_end, xdt)
    return S, y


def ssd_chunked(xdt, a, Bm, Cm):
    S0 = jnp.zeros((xdt.shape[0], SSD_GROUPS, SSD_HPG, SSD_HEADDIM, SSD_STATE), jnp.float32)
    _, y = lax.scan(ssd_chunk_step, S0, (to_chunks(xdt), to_chunks(a), to_chunks(Bm), to_chunks(Cm)))
    return from_chunks(y)


def hybrid_mixer(hn, w_in, gla_w_gate2, gla_b_gate, gla_norm_w, conv_w, conv_b, dt_bias, a_log, d_skip,
                 ssd_norm_w, w_up_gla, w_up_ssd, w_out):
    Bsz, L, _ = hn.shape
    f32 = jnp.float32
    proj = hn @ w_in
    q, k, v, g_out, g_low, z, xbc, dt_raw, gate_a, gate_b = split_cols(proj, IN_SPLITS)

    log_g = jax.nn.log_sigmoid((g_low @ gla_w_gate2 + gla_b_gate).astype(f32)) / GLA_GATE_NORM
    hd = lambda t, n: t.reshape(Bsz, L, GLA_HEADS, n)
    o = gla_chunked(pad_front(hd(q, GLA_DK) * GLA_DK ** -0.5), pad_front(hd(k, GLA_DK)),
                    pad_front(hd(v, GLA_DV)), pad_front(hd(log_g, GLA_DK)))[:, META_PAD:]
    o = rmsnorm(o, gla_norm_w) * jax.nn.silu(hd(g_out, GLA_DV))
    y_a = o.reshape(Bsz, L, GLA_VW).astype(hn.dtype) @ w_up_gla

    xbc = jax.nn.silu(causal_dwconv(xbc, conv_w, conv_b))
    xs, Bm, Cm = split_cols(xbc, (SSD_D_INNER, SSD_BC, SSD_BC))
    dt = jax.nn.softplus(dt_raw.astype(f32) + dt_bias)
    a = dt * (-jnp.exp(a_log.astype(f32)))
    grp = lambda t: t.reshape(Bsz, L, SSD_GROUPS, SSD_HPG)
    xs_h = xs.reshape(Bsz, L, SSD_GROUPS, SSD_HPG, SSD_HEADDIM)
    y = ssd_chunked(pad_front(xs_h * grp(dt)[..., None]), pad_front(grp(a)),
                    pad_front(Bm.reshape(Bsz, L, SSD_GROUPS, SSD_STATE)),
                    pad_front(Cm.reshape(Bsz, L, SSD_GROUPS, SSD_STATE)))[:, META_PAD:]
    y = y + d_skip.reshape(SSD_GROUPS, SSD_HPG)[:, :, None] * xs_h
    y = y.reshape(Bsz, L, SSD_D_INNER) * jax.nn.silu(z)
    y = rmsnorm(y.reshape(Bsz, L, SSD_GROUPS, SSD_D_INNER // SSD_GROUPS),
                ssd_norm_w.reshape(SSD_GROUPS, -1)).reshape(Bsz, L, SSD_D_INNER)
    y_b = y.astype(hn.dtype) @ w_up_ssd

    mixed = jax.nn.sigmoid(gate_a) * y_a + jax.nn.sigmoid(gate_b) * y_b
    return (mixed @ w_out).astype(hn.dtype)


def peer_ffn(h, w_q, sub_keys, u_tab, v_tab):
    T = h.shape[0]
    pad = (-T) % PEER_BLOCK
    hp = jnp.pad(h, ((0, pad), (0, 0))).reshape(-1, PEER_BLOCK, D_MODEL)

    def block(hb):
        n = hb.shape[0]
        q = (hb @ w_q).reshape(n, PEER_HEADS, 2, PEER_HALF)
        s = jnp.einsum('nhid,hikd->nhik', q, sub_keys).astype(jnp.float32)
        s_top, i_top = lax.top_k(s, PEER_TOPK)
        cand = s_top[:, :, 0, :, None] + s_top[:, :, 1, None, :]
        cand_idx = i_top[:, :, 0, :, None] * PEER_N_KEYS + i_top[:, :, 1, None, :]
        best, pos = lax.top_k(cand.reshape(n, PEER_HEADS, PEER_TOPK * PEER_TOPK), PEER_TOPK)
        idx = jnp.take_along_axis(cand_idx.reshape(n, PEER_HEADS, -1), pos, axis=-1)
        w = jax.nn.softmax(best, axis=-1)
        u = jnp.take(u_tab, idx, axis=0)
        act = jax.nn.gelu(jnp.einsum('nhkd,nd->nhk', u, hb).astype(jnp.float32))
        out = jnp.einsum('nhk,nhkd->nd', w * act, jnp.take(v_tab, idx, axis=0))
        return out.astype(hb.dtype)

    return lax.map(block, hp).reshape(-1, D_MODEL)[:T]


def setup_inputs(seed: int = 0) -> dict:
    key = jax.random.key(seed)
    ks = jax.random.split(key, 24)
    f32 = jnp.float32
    nrm = lambda k, shape, std: jax.random.normal(k, shape, f32) * std
    gain = lambda k, shape: 1.0 + 0.02 * jax.random.normal(k, shape, f32)
    Dp = DEPTH
    dt0 = jnp.exp(jax.random.uniform(ks[9], (Dp, SSD_HEADS), f32, math.log(1e-3), math.log(1e-1)))
    dt_bias = dt0 + jnp.log(-jnp.expm1(-dt0))
    a_log = jnp.log(jax.random.uniform(ks[10], (Dp, SSD_HEADS), f32, 1.0, 16.0))
    return {
        'x': nrm(ks[0], (BATCH, SEQ, D_MODEL), 1.0),
        'meta_tokens': nrm(ks[1], (N_META, D_MODEL), 1.0),
        'ln_mix_w': gain(ks[2], (Dp, D_MODEL)),
        'w_in': nrm(ks[3], (Dp, D_MODEL, IN_WIDTH), D_MODEL ** -0.5),
        'gla_w_gate2': nrm(ks[4], (Dp, GLA_GATE_RANK, GLA_QK), GLA_GATE_RANK ** -0.5),
        'gla_b_gate': nrm(ks[5], (Dp, GLA_QK), 0.1),
        'gla_norm_w': gain(ks[6], (Dp, GLA_DV)),
        'ssd_conv_w': nrm(ks[7], (Dp, SSD_CONV, SSD_XBC), SSD_CONV ** -0.5),
        'ssd_conv_b': nrm(ks[8], (Dp, SSD_XBC), 0.02),
        'ssd_dt_bias': dt_bias,
        'ssd_a_log': a_log,
        'ssd_d': gain(ks[11], (Dp, SSD_HEADS)),
        'ssd_norm_w': gain(ks[12], (Dp, SSD_D_INNER)),
        'w_up_gla': nrm(ks[13], (Dp, GLA_VW, D_MODEL), GLA_VW ** -0.5),
        'w_up_ssd': nrm(ks[14], (Dp, SSD_D_INNER, D_MODEL), SSD_D_INNER ** -0.5),
        'w_out': nrm(ks[15], (Dp, D_MODEL, D_MODEL), D_MODEL ** -0.5),
        'ln_ffn_w': gain(ks[16], (Dp, D_MODEL)),
        'peer_w_q': nrm(ks[17], (Dp, D_MODEL, PEER_HEADS * PEER_KEY_DIM), D_MODEL ** -0.5),
        'peer_sub_keys': nrm(ks[18], (Dp, PEER_HEADS, 2, PEER_N_KEYS, PEER_HALF), PEER_HALF ** -0.5),
        'peer_u': nrm(ks[19], (Dp, PEER_N_EXPERTS, D_MODEL), D_MODEL ** -0.5),
        'peer_v': nrm(ks[20], (Dp, PEER_N_EXPERTS, D_MODEL), PEER_HEADS ** -0.5),
        'ln_final_w': gain(ks[21], (D_MODEL,)),
    }


def reference(x, meta_tokens, ln_mix_w, w_in, gla_w_gate2, gla_b_gate, gla_norm_w, ssd_conv_w, ssd_conv_b,
              ssd_dt_bias, ssd_a_log, ssd_d, ssd_norm_w, w_up_gla, w_up_ssd, w_out, ln_ffn_w,
              peer_w_q, peer_sub_keys, peer_u, peer_v, ln_final_w):
    Bsz = x.shape[0]
    meta = jnp.broadcast_to(meta_tokens[None].astype(x.dtype), (Bsz, N_META, D_MODEL))
    h = jnp.concatenate([meta, x], axis=1)
    for l in range(DEPTH):
        hn = rmsnorm(h, ln_mix_w[l])
        h = h + hybrid_mixer(hn, w_in[l], gla_w_gate2[l], gla_b_gate[l], gla_norm_w[l], ssd_conv_w[l],
                             ssd_conv_b[l], ssd_dt_bias[l], ssd_a_log[l], ssd_d[l], ssd_norm_w[l],
                             w_up_gla[l], w_up_ssd[l], w_out[l])
        if l == DEPTH - 1:
            h = h[:, N_META:]
        hn = rmsnorm(h, ln_ffn_w[l])
        ffn = peer_ffn(hn.reshape(-1, D_MODEL), peer_w_q[l], peer_sub_keys[l], peer_u[l], peer_v[l])
        h = h + ffn.reshape(h.shape)
    return rmsnorm(h, ln_final_w)
```

```python
import contextlib
import numpy as np
import concourse.bass as bass
import concourse.mybir as mybir
from concourse.bass_utils import run_bass_kernel_spmd

F32 = mybir.dt.float32
F32R = mybir.dt.float32
I32 = mybir.dt.int32
BF16 = mybir.dt.bfloat16
U32 = mybir.dt.uint32
AF = mybir.ActivationFunctionType
ALU = mybir.AluOpType
AX = mybir.AxisListType

ENGS = ("pe", "act", "dve", "pool", "sp")


class Sched:
    def __init__(self, nc):
        self.nc = nc
        self.ops = {e: [] for e in ENGS}
        self.count = {e: 0 for e in ENGS}
        self.known = {e: {} for e in ENGS}
        self.last_write = {}
        self.readers = {}
        self.dma_cnt = {}
        self.overlaps = {}
        self.semkeys = set()
        self.seq = 0
        self.labels = []
        self.limit = None
        self.pump = None
        self.pump_rate = 4
        self._pc = 0
        self._inpump = False
    EPOCH = 3000
    DEPOCH = 187

    def _deps(self, eng, reads, writes):
        deps = {}

        def add(d, same_ok):
            if d is None:
                return
            s, v = d
            if s[0] == eng and not same_ok:
                return
            if deps.get(s, 0) < v:
                deps[s] = v
        for r in reads:
            add(self.last_write.get(r), eng != "pe")
        for w in writes:
            for k in [w] + self.overlaps.get(w, []):
                add(self.last_write.get(k), eng != "pe")
                for d in self.readers.get(k, ()):
                    add(d, eng != "pe")
        waits = []
        kn = self.known[eng]
        for s, v in deps.items():
            if kn.get(s, 0) < v:
                kn[s] = v
                waits.append((s, v))
        return waits

    def _commit(self, tag, reads, writes):
        for r in reads:
            self.readers.setdefault(r, []).append(tag)
        for w in writes:
            self.last_write[w] = tag
            self.readers[w] = []

    @staticmethod
    def _psx(reads, writes):
        pr = [r for r in reads if r.startswith("ps")]
        if not pr:
            return list(reads), list(writes)
        return [r for r in reads if not r.startswith("ps")], list(writes) + [r for r in pr if r not in writes]

    def _maybe_pump(self):
        if self.pump is None or self._inpump:
            return
        self._pc += 1
        if self._pc % self.pump_rate:
            return
        self._inpump = True
        try:
            next(self.pump)
        except StopIteration:
            self.pump = None
        self._inpump = False

    def drain(self):
        if self.pump is None:
            return
        self._inpump = True
        for _ in self.pump:
            pass
        self._inpump = False
        self.pump = None

    def op(self, eng, fn, reads=(), writes=()):
        self._maybe_pump()
        reads, writes = self._psx(reads, writes)
        waits = self._deps(eng, reads, writes)
        self.count[eng] += 1
        idx = self.count[eng]
        tag = ((eng,), idx)
        self.ops[eng].append((fn, waits, ("eng", eng, idx), self.seq))
        self._note(eng)
        self._commit(tag, reads, writes)

    def dma(self, q, fn, semkey, reads=(), writes=()):
        self._maybe_pump()
        waits = self._deps(q, reads, writes)
        self.dma_cnt[semkey] = self.dma_cnt.get(semkey, 0) + 1
        c = self.dma_cnt[semkey] - 1
        sk = ("dma", semkey, c // self.DEPOCH)
        tag = (sk, 16 * (c % self.DEPOCH + 1))
        self.semkeys.add(sk)
        self.ops[q].append((fn, waits, (sk, 16), self.seq))
        self._note(q + "-dma")
        self._commit(tag, reads, writes)

    def seal(self, semkey, keys):
        sk = ("dma", semkey, 0)
        for k in keys:
            self.last_write[k] = (sk, 16 * self.dma_cnt[semkey])

    def wait_all(self, eng, keys):
        waits = self._deps(eng, list(keys), ())
        self.ops[eng].append((None, waits, None, self.seq))
        self._note(eng + "-waitall")

    def _note(self, what):
        import sys as _s
        f = _s._getframe(2)
        ln = []
        for _ in range(4):
            if f is None:
                break
            ln.append(f.f_lineno)
            f = f.f_back
        self.labels.append((what, ln))
        self.seq += 1

    def emit(self):
        nc = self.nc
        waited = {e: set() for e in ENGS}
        for e in ENGS:
            for fn, waits, inc, seq in self.ops[e]:
                for sk, v in waits:
                    if len(sk) == 1:
                        waited[sk[0]].add(v)
        sig = {}
        semkeys = set(self.semkeys)
        for e in ENGS:
            c = 0
            for fn, waits, inc, seq in self.ops[e]:
                if inc is not None and inc[0] == "eng" and inc[2] in waited[e]:
                    sig[(e, inc[2])] = ((e, c // self.EPOCH), c % self.EPOCH + 1)
                    semkeys.add((e, c // self.EPOCH))
                    c += 1
        self.n_signals = len(sig)

        def tr(sk, v):
            return sig[(sk[0], v)] if len(sk) == 1 else (sk, v)
        with contextlib.ExitStack() as st:
            sems = {}
            for i, k in enumerate(sorted(semkeys, key=str)):
                sems[k] = st.enter_context(nc.semaphore("s%d" % i))
            block = st.enter_context(nc.Block())
            engmap = {"pe": block.tensor, "act": block.scalar, "dve": block.vector,
                      "pool": block.gpsimd, "sp": block.sync}
            for e in ENGS:
                ops = self.ops[e]
                if not ops:
                    continue

                def body(eng, ops=ops):
                    for fn, waits, inc, seq in ops:
                        if self.limit is not None and seq >= self.limit:
                            break
                        for s, v in waits:
                            s2, v2 = tr(s, v)
                            eng.wait_ge(sems[s2], v2)
                        if fn is not None:
                            ins = fn(eng)
                            if inc is not None:
                                if inc[0] == "eng":
                                    sg_ = sig.get((inc[1], inc[2]))
                                    if sg_ is not None:
                                        ins.then_inc(sems[sg_[0]], 1)
                                else:
                                    ins.then_inc(sems[inc[0]], inc[1])
                engmap[e](body)
        return nc


D = 1024
INW = 10288
C_Q, C_K, C_V, C_GO, C_GL, C_Z, C_XBC, C_DT, C_GA, C_GB = 0, 512, 1024, 2048, 3072, 3088, 5136, 8208, 8240, 9264
NCONST = 9
EPS = 1e-6


def make_consts():
    p = np.arange(128)[:, None]
    f = np.arange(128)[None, :]
    same = (p // 64) == (f // 64)
    c = np.zeros((128, NCONST + 1, 128), np.float32)
    c[:, 0] = (p == f)
    c[:, 1] = np.where(same & (p <= f), -1.0 / 16, 0.0)
    c[:, 2] = np.where(same & (p > f), -1.0 / 16, 0.0)
    c[:, 3] = (same & (p <= f))
    c[:, 4] = (same & (p > f))
    c[:, 5] = (p // 64 == 0) * np.ones((1, 128))
    c[:, 6] = (p // 64 == 1) * np.ones((1, 128))
    c[:, 7] = np.ones((128, 1)) * (f // 64 == 0)
    c[:, 8] = np.ones((128, 1)) * (f // 64 == 1)
    c[:, 9, 0:64] = ((p % 64) <= np.arange(64)[None, :])
    c[:, 9, 64:80] = np.arange(16)[None, :]
    return c.reshape(128, (NCONST + 1) * 128)


XPERM = list(range(0, 8)) + [16, 17, 20, 21] + list(range(8, 16)) + [18, 19, 22, 23]


def build(NPT, NMT, dbg=False, limit=None):
    nc = bass.Bass("TRN2", target_bir_lowering=False)
    NP, NM = NPT * 128, NMT * 128

    def din(name, shape, dt=F32):
        return nc.dram_tensor(name, shape, dt, kind="ExternalInput").ap()

    xpT = din("xpT", [D, NP]); xmT = din("xmT", [D, NM]); maskp_d = din("maskp", [128, NPT])
    w_in = din("w_in", [D, INW]); w2aug_d = din("w2aug", [17, 512])
    w_up_gla = din("w_up_gla", [1024, D]); w_up_ssd = din("w_up_ssd", [2048, D]); w_out = din("w_out", [D, D])
    peer_wq = din("peer_wq", [D, 2048]); skT_d = din("skT", [128, 16 * 128])
    peer_u = din("peer_u", [16384, D]); peer_v = din("peer_v", [16384, D])
    consts_d = din("consts", [128, (NCONST + 1) * 128])
    NSA = 8 + 8 + 96 + 24 + 2 + 16
    smallA_d = din("smallA", [128, NSA])
    smallB_d = din("smallB", [128, 1024 + 96])
    out_d = nc.dram_tensor("out", [NM, D], F32, kind="ExternalOutput").ap()
    WB = {}
    wlist = []

    def add_w(wname, wap, KC, segs):
        CWmax = 512 if KC == 8 else 256
        for (c0, n) in segs:
            for o in range(0, n, CWmax):
                cw = min(CWmax, n - o)
                WB[(wname, c0 + o)] = (len(wlist), KC, cw)
                wlist.append((wap, KC, c0 + o, cw))
    add_w("w_in", w_in, 8, [(C_Q, 512), (C_K, 512), (C_V, 1024), (C_GO, 1024), (C_GL, 16), (C_Z, 2048), (C_XBC, 3072), (C_DT, 32),
                            (C_GA, 1024), (C_GB, 1024)])
    add_w("w_up_gla", w_up_gla, 8, [(0, 1024)])
    add_w("w_up_ssd", w_up_ssd, 16, [(0, 1024)])
    add_w("w_out", w_out, 8, [(0, 1024)])
    add_w("peer_wq", peer_wq, 8, [(0, 2048)])
    wscr = nc.dram_tensor("wscr", [len(wlist), 128, 4096], BF16, kind="Internal").ap()
    u_bf = nc.dram_tensor("u_bf", [16384, D], BF16, kind="Internal").ap()
    v_bf = nc.dram_tensor("v_bf", [16384, D], BF16, kind="Internal").ap()
    if dbg:
        dbg_h1T = nc.dram_tensor("dbg_h1T", [D, NM], F32, kind="ExternalOutput").ap()

    with contextlib.ExitStack() as st:
        def T(name, shape, dt=F32):
            return st.enter_context(nc.sbuf_tensor("sb_" + name, shape, dt))
        ps = [st.enter_context(nc.psum_tensor("ps%d" % i, [128, 512], F32)) for i in range(8)]
        PK = ["ps%d" % i for i in range(8)]
        S = Sched(nc)

        consts = T("consts", [128, (NCONST + 1) * 128], F32R)
        cF = consts[:].bitcast(F32)

        def CR(i):
            return consts[:, i * 128:(i + 1) * 128]

        def CF(i):
            return cF[:, i * 128:(i + 1) * 128]
        ident = CF(0)
        triL = cF[:, 9 * 128:9 * 128 + 64]
        iota16 = cF[:, 9 * 128 + 64:9 * 128 + 80]
        smallA = T("smallA", [128, NSA]); smallB = T("smallB", [128, 1120])
        lnmix = smallA[:, 0:8]; lnffn = smallA[:, 8:16]
        convw = smallA[:, 16:112].rearrange("p (c i) -> p c i", i=4); convb = smallA[:, 112:136]
        gnw_fm = smallA[:, 136:138]; snw_fm = smallA[:, 138:154]
        lnf_bc = smallB[:, 0:1024]
        dtb_bc = smallB[:, 1024:1056]; alog_bc = smallB[:, 1056:1088]; dsk_bc = smallB[:, 1088:1120]
        maskp = T("maskp", [128, NPT]); w2aug = T("w2aug", [17, 512], F32R); skT = T("skT", [128, 16, 128], F32R)
        negA = T("negA", [128, 32]); ones = T("ones", [128, 128], F32R)
        bd_ones = T("bd_ones", [128, 128], F32R); negC = T("negC", [128, 128], F32R)

        S.dma("sp", lambda e: e.dma_start(out=smallA[:], in_=smallA_d), "setup", writes=["smallA"])
        S.dma("sp", lambda e: e.dma_start(out=smallB[:], in_=smallB_d), "setup", writes=["smallB"])
        S.dma("sp", lambda e: e.dma_start(out=maskp[:], in_=maskp_d), "setup", writes=["maskp"])
        S.dma("pool", lambda e: e.dma_start(out=consts[:], in_=consts_d), "setupc", writes=["consts"])
        S.dma("pool", lambda e: e.dma_start(out=w2aug[:], in_=w2aug_d), "setupc", writes=["w2aug"])
        S.dma("pool", lambda e: e.dma_start(out=skT[:].rearrange("p a b -> p (a b)"), in_=skT_d), "setupc", writes=["skT"])
        S.seal("setup", ["smallA", "smallB", "maskp"])
        S.seal("setupc", ["consts", "w2aug", "skT"])
        def _early(bi):
            wap, KC, c0, cw = wlist[bi]
            return wap is w_in and ((C_K <= c0 < C_GO) or (C_GL <= c0 < C_Z) or (C_XBC <= c0 < C_GA))
        wgrp = {bi: (0 if _early(bi) else 1) for bi in range(len(wlist))}

        def conv_weights(grp):
            for bi, (wap, KC, c0, cw) in enumerate(wlist):
                if wgrp[bi] != grp:
                    continue
                dst = wscr[bi][:, 0:KC * cw].rearrange("p (k c) -> p k c", k=KC)
                src = wap.rearrange("(k p) c -> p k c", p=128)[:, :, c0:c0 + cw]
                S.dma("pool", lambda e, dst=dst, src=src: e.dma_start(out=dst, in_=src), "cv%d" % grp, writes=["wscr%d" % grp])
                yield
        for _ in conv_weights(0):
            pass
        S.seal("cv0", ["wscr0"])
        CR_ = 512

        def conv_tables():
            for _ in conv_weights(1):
                yield
            for (src_t, dst_t) in ((peer_u, u_bf), (peer_v, v_bf)):
                for r0 in range(0, 16384, CR_):
                    S.dma("pool", lambda e, src_t=src_t, dst_t=dst_t, r0=r0: e.dma_start(out=dst_t[r0:r0 + CR_, :], in_=src_t[r0:r0 + CR_, :]),
                          "cvt", writes=["uvbf"])
                    yield

        Sg = T("Sg", [128, 4, 256], F32R)
        ST = T("ST", [128, 2048], F32R)
        xbc = T("xbc", [128, 24, 131])
        MT = T("MT", [128, 16, 128], F32R)
        glaug = T("glaug", [32, 128], F32R)
        S.op("dve", lambda e: e.memset(Sg[:], 0.0), writes=["Sg0", "Sg1", "Sg2", "Sg3"])
        S.op("dve", lambda e: e.memset(ST[:], 0.0), writes=["ST0", "ST1", "ST2", "ST3"])
        S.op("dve", lambda e: e.memset(xbc[:], 0.0), writes=["xbc"])
        S.op("dve", lambda e: e.memset(MT[:], 0.0), writes=["MT"])
        S.op("dve", lambda e: e.memset(glaug[:], 1.0), writes=["glaug"])
        S.op("dve", lambda e: e.memset(ones[:], 1.0), writes=["ones"])
        S.op("act", lambda e: e.activation(out=negA[:], in_=alog_bc, func=AF.Exp), reads=["smallB"], writes=["negA"])
        S.op("dve", lambda e: e.tensor_scalar(out=negA[:], in0=negA[:], scalar1=-1.0, scalar2=None, op0=ALU.mult),
             reads=["negA"], writes=["negA"])
        S.op("dve", lambda e: e.tensor_tensor(out=bd_ones[:], in0=CF(3), in1=CF(4), op=ALU.add), ["consts"], ["bd"])
        S.op("dve", lambda e: e.tensor_scalar(out=negC[:], in0=CF(3), scalar1=-1.0, scalar2=None, op0=ALU.mult), ["consts"], ["bd"])

        NWB = 3
        wbt = [T("wb%d" % i, [128, 4096], BF16) for i in range(NWB)]
        wsel = [0]
        xT = [T("xT0", [128, 8, 128])]
        hnT = T("hnT", [128, 8, 128], BF16)
        sq = T("sq", [128, 8, 128], F32R)
        h1T = T("h1T", [128, 8, 128])
        rstd_bc = T("rstd_bc", [128, 128])
        st8 = T("st8", [128, 16])
        dt_sb = T("dt_sb", [128, 32]); a_sb = T("a_sb", [128, 32], F32R); ex4 = T("ex4", [128, 4, 32])

        ARW = 20736
        AR = T("arena", [128, ARW])
        abufs = {}

        def A(name, off, shape, dt=F32):
            size = 1
            for d_ in shape[1:]:
                size *= d_
            if dt == BF16:
                size //= 2
            assert off + size <= ARW, (name, off, size)
            ap = AR[0:shape[0], off:off + size]
            if dt != F32:
                ap = ap.bitcast(dt)
            if len(shape) == 3:
                ap = ap.rearrange("p (a b) -> p a b", a=shape[1])
            elif len(shape) == 4:
                ap = ap.rearrange("p (a b c) -> p a b c", a=shape[1], b=shape[2])
            abufs[name] = (off, size)
            return ap
        qT = A("qT", 0, [128, 4, 128]); kT = A("kT", 512, [128, 4, 128]); k_tok = A("k_tok", 1024, [128, 512])
        v_tok = A("v_tok", 1536, [128, 1024], F32R); lgp = A("lgp", 2560, [128, 512], F32R)
        oT = A("oT", 3072, [128, 8, 128], BF16); yT = A("yT", 3584, [128, 16, 128], BF16); mixT = A("mixT", 4608, [128, 8, 128], BF16)
        X = 5120
        eG = A("eG", X, [128, 4, 128]); eGn = A("eGn", X + 512, [128, 4, 128])
        qe = A("qe", X + 1024, [128, 4, 128], F32R); qn = A("qn", X + 1536, [128, 4, 128], F32R)
        ke = A("ke", X + 2048, [128, 4, 128], F32R); kn = A("kn", X + 2560, [128, 4, 128], F32R)
        qe0 = A("qe0", X + 3072, [128, 4, 128], F32R); qe1 = A("qe1", X + 3584, [128, 4, 128], F32R)
        attC = A("attC", X + 4096, [128, 4, 128], F32R); attA = A("attA", X + 4608, [128, 4, 128], F32R)
        kdec = A("kdec", X + 5120, [128, 512], F32R); tmp512 = A("tmp512", X + 5632, [128, 512])
        acc = A("acc", X, [128, 12, 128]); acc2 = A("acc2", X + 1536, [128, 12, 128])
        R1 = A("R1", X, [128, 16, 64], F32R); R2 = A("R2", X + 1024, [128, 16, 64], F32R); seg = A("seg", X + 2048, [128, 1024])
        m1 = A("m1", X + 3072, [128, 512]); m2 = A("m2", X + 3584, [128, 512])
        ga = A("ga", X + 4096, [128, 4, 128]); gb = A("gb", X + 4608, [128, 4, 128])
        Y = 11264
        xsT = A("xsT", Y, [128, 8, 128]); bcT = A("bcT", Y + 1024, [128, 4, 128], F32R)
        xdt = A("xdt", Y + 1536, [128, 1024], F32R); xdte = A("xdte", Y + 2560, [128, 1024], F32R)
        xs_tok = A("xs_tok", Y + 3584, [128, 1024]); B_tok = A("B_tok", Y + 4608, [128, 256], F32R)
        Cm0 = A("Cm0", Y + 4864, [128, 2, 128], F32R); Cm1 = A("Cm1", Y + 5120, [128, 2, 128], F32R)
        yi = A("yi", Y + 5376, [128, 1024]); y_sb = A("y_sb", Y + 6400, [128, 1024]); t2 = A("t2", Y + 7424, [128, 1024])
        sz = A("sz", Y + 8448, [128, 1024])
        wb_extra = [A("wb2", Y + 5376, [128, 4096], BF16), A("wb3", Y + 7424, [128, 4096], BF16),
                    A("wb4", 3072, [128, 4096], BF16), A("wb5", X + 3072, [128, 4096], BF16)]
        prefix_mode = [False]
        o_sb = A("o_sb", Y, [128, 1024]); sg = A("sg", Y + 1024, [128, 1024])
        pqT = A("pqT", 0, [128, 16, 128], F32R); sc = A("sc", 2048, [128, 16, 128]); cand = A("cand", 4096, [128, 8, 256])
        oh = A("oh", 6144, [128, 8, 16, 16])
        po = [8192]

        def AS(name, words, shape, dt=F32):
            ap = A(name, po[0], shape, dt)
            po[0] += words
            return ap
        work = AS("work", 256, [128, 256]); stv = AS("stv", 256, [128, 16, 16]); sti = AS("sti", 256, [128, 16, 16], U32)
        sif = AS("sif", 256, [128, 16, 16]); best = AS("best", 128, [128, 8, 16]); pos = AS("pos", 128, [128, 8, 16], U32)
        posa = AS("posa", 128, [128, 8, 16], U32); posb = AS("posb", 128, [128, 8, 16], U32)
        af = AS("af", 128, [128, 8, 16]); bf = AS("bf", 128, [128, 8, 16])
        i0s = AS("i0s", 128, [128, 8, 16]); i1s = AS("i1s", 128, [128, 8, 16])
        eidx_f = AS("eidx_f", 128, [128, 128]); ssum = AS("ssum", 8, [128, 8])
        h1toks = [T("h1tok%d" % i, [128, 1024])[:] for i in range(2)]
        hn2toks = [T("hn2b%d" % i, [128, 1024], BF16)[:] for i in range(2)]
        NSLOT = 8
        slots = [T("gs%d" % i, [128, 1024], BF16)[:] for i in range(NSLOT)]
        NDG = 4
        dgs = [T("dg%d" % i, [128, 128], BF16)[:] for i in range(NDG)]
        eidxs = [T("eidx%d" % i, [128, 128], I32)[:] for i in range(2)]
        wsms = [T("wsm%d" % i, [128, 8, 16])[:] for i in range(2)]
        pre = T("pre", [128, 128])[:]; gtmp = T("gtmp", [128, 128])[:]; coef = T("coef", [128, 128])[:]; st9 = T("st9", [128, 4])[:]
        names = list(abufs)
        for n1 in names:
            o1, s1 = abufs[n1]
            S.overlaps[n1] = [n2 for n2 in names if n2 != n1 and abufs[n2][0] < o1 + s1 and o1 < abufs[n2][0] + abufs[n2][1]]

        def MM(out, lhsT, rhs, start, stop, r, w):
            S.op("pe", lambda e: e.matmul(out, lhsT=lhsT, rhs=rhs, start=start, stop=stop), r, w)

        def TR(out, in_, r, w):
            S.op("pe", lambda e: e.transpose(out, in_, ident), list(r) + ["consts"], w)

        def ACT(out, in_, func, r, w, bias=None, scale=None, accum=None):
            kw = {}
            if bias is not None:
                kw["bias"] = bias
            if scale is not None:
                kw["scale"] = scale
            if accum is not None:
                kw["accum_out"] = accum
            S.op("act", lambda e: e.activation(out=out, in_=in_, func=func, **kw), r, w)

        def TTo(eng, out, in0, in1, op, r, w):
            S.op(eng, lambda e: e.tensor_tensor(out=out, in0=in0, in1=in1, op=op), r, w)

        def TS(eng, out, in0, s1, op0, r, w, s2=None, op1=None):
            if op1 is None:
                S.op(eng, lambda e: e.tensor_scalar(out=out, in0=in0, scalar1=s1, scalar2=None, op0=op0), r, w)
            else:
                S.op(eng, lambda e: e.tensor_scalar(out=out, in0=in0, scalar1=s1, scalar2=s2, op0=op0, op1=op1), r, w)

        def STT(eng, out, in0, scalar, in1, op0, op1, r, w, accum=None):
            if accum is None:
                S.op(eng, lambda e: e.scalar_tensor_tensor(out=out, in0=in0, scalar=scalar, in1=in1, op0=op0, op1=op1), r, w)
            else:
                S.op(eng, lambda e: e.scalar_tensor_tensor(out=out, in0=in0, scalar=scalar, in1=in1, op0=op0, op1=op1,
                                                           accum_out=accum), r, w)

        def CP(eng, out, in_, r, w):
            if eng == "act":
                S.op("act", lambda e: e.copy(out=out, in_=in_), r, w)
            else:
                S.op(eng, lambda e: e.tensor_copy(out=out, in_=in_), r, w)

        def f2(ap):
            return ap.rearrange("p a b -> p (a b)")

        def load_w(wname, c0):
            bi, KC, cw = WB[(wname, c0)]
            nb_ = NWB + len(wb_extra) if prefix_mode[0] else NWB
            i = wsel[0] % nb_
            wsel[0] = (i + 1) % nb_
            flat = (wbt[i][:, 0:KC * cw] if i < NWB else wb_extra[i - NWB][:, 0:KC * cw])
            view = flat.rearrange("p (k c) -> p k c", k=KC)
            src = wscr[bi][:, 0:KC * cw]
            key = "wb%d" % i
            S.dma("sp", lambda e: e.dma_start(out=flat, in_=src), key, reads=["wscr%d" % wgrp[bi]], writes=[key])
            return view, key, KC, cw

        pj = [0]

        def proj_bank():
            pj[0] ^= 1
            return pj[0]

        def rstd_from(out, in_, n, r, w):
            ACT(out, in_, AF.Ln, r, w, bias=EPS, scale=1.0 / n)
            ACT(out, out, AF.Exp, w, w, scale=-0.5)

        def norm_fm(srcT, srckey, lnw, dstT, dstkey, f32copy=False):
            ACT(f2(sq[:]), f2(srcT), AF.Square, [srckey], ["sq"])
            b = 2
            for k in range(8):
                MM(ps[b][:, 0:128], ones[:], sq[:, k, :], k == 0, k == 7, ["ones", "sq"], [PK[b]])
            rstd_from(rstd_bc[:], ps[b][:, 0:128], 1024.0, [PK[b]], ["rstd_bc"])
            for k in range(8):
                STT("dve", dstT[:, k, :], srcT[:, k, :], lnw[:, k:k + 1], rstd_bc[:], ALU.mult, ALU.mult,
                    [srckey, "rstd_bc", "smallA"], [dstkey])
            if f32copy:
                for k in range(8):
                    STT("dve", sq[:, k, :], srcT[:, k, :], lnw[:, k:k + 1], rstd_bc[:], ALU.mult, ALU.mult,
                        [srckey, "rstd_bc", "smallA"], ["sq"])

        def fm_proj(wname, c0, ncols, actT, akey, evac):
            o = 0
            while o < ncols:
                wv, wk, KC, cw = load_w(wname, c0 + o)
                b = proj_bank()
                sw = min(cw, 128)
                for sub in range(max(1, cw // 128)):
                    for k in range(KC):
                        MM(ps[b][0:sw, sub * 128:(sub + 1) * 128], wv[:, k, sub * sw:(sub + 1) * sw], actT[:, k, :],
                           k == 0, k == KC - 1, [wk, akey], [PK[b]])
                evac(b, o, cw)
                o += cw

        def tm_proj(wname, c0, ncols, evac):
            o = 0
            while o < ncols:
                wv, wk, KC, cw = load_w(wname, c0 + o)
                b = proj_bank()
                for k in range(8):
                    MM(ps[b][:, 0:cw], hnT[:, k, :], wv[:, k, :], k == 0, k == 7, [wk, "hnT"], [PK[b]])
                evac(b, o, cw)
                o += cw

        def load_x(xsrc, ti):
            srcv = xsrc.rearrange("(k p) t -> p k t", p=128)[:, :, ti * 128:(ti + 1) * 128]
            S.dma("sp", lambda e: e.dma_start(out=xT[0][:], in_=srcv), "xT0", writes=["xT0"])

        def mixer_tile(xsrc, ti, full, mask_ap, xi, last_prefix=False, xloaded=False, next_x=None):
            xt = xT[xi]; xkey = "xT%d" % xi
            if not xloaded:
                load_x(xsrc, ti)
            norm_fm(xt[:], xkey, lnmix, hnT, "hnT")
            if next_x is not None and not full:
                load_x(*next_x)
            mkeys = ["maskp"] if mask_ap is not None else []
            PL = "dve"
            if full:
                fm_proj("w_in", C_Q, 512, hnT, "hnT",
                        lambda b, o, cw: S.op("act", lambda e: e.mul(out=f2(qT[:, o // 128:o // 128 + cw // 128, :]), in_=ps[b][:, 0:cw], mul=128.0 ** -0.5),
                                              [PK[b]], ["qT"]))
                fm_proj("w_in", C_K, 512, hnT, "hnT", lambda b, o, cw: CP("act", f2(kT[:, o // 128:o // 128 + cw // 128, :]), ps[b][:, 0:cw], [PK[b]], ["kT"]))
            if mask_ap is None:
                tm_proj("w_in", C_K, 512, lambda b, o, cw: CP("dve", k_tok[:, o:o + cw], ps[b][:, 0:cw], [PK[b]], ["k_tok"]))
            else:
                tm_proj("w_in", C_K, 512, lambda b, o, cw: TS("dve", k_tok[:, o:o + cw], ps[b][:, 0:cw], mask_ap, ALU.mult, [PK[b]] + mkeys, ["k_tok"]))
            tm_proj("w_in", C_V, 1024, lambda b, o, cw: CP("act", v_tok[:, o:o + cw], ps[b][:, 0:cw], [PK[b]], ["v_tok"]))
            fm_proj("w_in", C_GL, 16, hnT, "hnT", lambda b, o, cw: CP("dve", glaug[0:16, :], ps[b][0:16, 0:128], [PK[b]], ["glaug"]))
            b = 3
            MM(ps[b][:, :], glaug[0:17, :], w2aug[:, :], True, True, ["glaug", "w2aug"], [PK[b]])
            ACT(tmp512[:], ps[b][:, :], AF.Exp, [PK[b]], ["tmp512"], scale=-1.0)
            if mask_ap is None:
                ACT(lgp[:], tmp512[:], AF.Ln, ["tmp512"], ["lgp"], bias=1.0)
            else:
                ACT(tmp512[:], tmp512[:], AF.Ln, ["tmp512"], ["tmp512"], bias=1.0)
                TS("dve", lgp[:], tmp512[:], mask_ap, ALU.mult, ["tmp512"] + mkeys, ["lgp"])
            def ev_xbc(b, o, cw):
                c0_ = o // 128
                for j in range(cw // 128):
                    slot = XPERM.index(c0_ + j)
                    CP("act" if j else "dve", xbc[:, slot, 3:131], ps[b][:, j * 128:(j + 1) * 128], [PK[b]], ["xbc"])
            fm_proj("w_in", C_XBC, 3072 if (full or last_prefix) else 2560, hnT, "hnT", ev_xbc)

            def ev_dt(b, o, cw):
                TTo("dve", dt_sb[:], ps[b][:, 0:32], dtb_bc, ALU.add, [PK[b], "smallB"], ["dt_sb"])
                ACT(dt_sb[:], dt_sb[:], AF.Exp, ["dt_sb"], ["dt_sb"])
                ACT(dt_sb[:], dt_sb[:], AF.Ln, ["dt_sb"], ["dt_sb"], bias=1.0)
                if mask_ap is not None:
                    TS("dve", dt_sb[:], dt_sb[:], mask_ap, ALU.mult, ["dt_sb"] + mkeys, ["dt_sb"])
                TTo("dve", a_sb[:], dt_sb[:], negA[:], ALU.mult, ["dt_sb", "negA"], ["a_sb"])
            tm_proj("w_in", C_DT, 32, ev_dt)

            b = 2
            for h in range(4):
                MM(ps[b][:, h * 128:(h + 1) * 128], lgp[:, h * 128:(h + 1) * 128], CR(1), True, True, ["lgp", "consts"], [PK[b]])
            ACT(f2(eG[:]), ps[b][:, :], AF.Exp, [PK[b]], ["eG"])
            if full:
                ACT(f2(eGn[:]), ps[b][:, :], AF.Exp, [PK[b]], ["eGn"], scale=-1.0)
                TTo("dve", qe[:], qT[:], eG[:], ALU.mult, ["qT", "eG"], ["qe"])
                TTo("dve", qn[:], qT[:], eGn[:], ALU.mult, ["qT", "eGn"], ["qn"])
                TTo("dve", ke[:], kT[:], eG[:], ALU.mult, ["kT", "eG"], ["ke"])
                TTo("dve", kn[:], kT[:], eGn[:], ALU.mult, ["kT", "eGn"], ["kn"])
                TTo("dve", qe0[:], qe[:].bitcast(F32), CF(7).unsqueeze(1).to_broadcast([128, 4, 128]), ALU.mult, ["qe", "consts"], ["qe0"])
                TTo("dve", qe1[:], qe[:].bitcast(F32), CF(8).unsqueeze(1).to_broadcast([128, 4, 128]), ALU.mult, ["qe", "consts"], ["qe1"])
                for h in range(4):
                    MM(ps[4][:, h * 128:(h + 1) * 128], kn[:, h, :], qe[:, h, :], True, True, ["kn", "qe"], [PK[4]])
                    MM(ps[5][:, h * 128:(h + 1) * 128], ke[:, h, :], qn[:, h, :], True, True, ["ke", "qn"], [PK[5]])
                TTo("dve", attC[:], ps[4][:, :].rearrange("p (h t) -> p h t", h=4), CF(3).unsqueeze(1).to_broadcast([128, 4, 128]),
                    ALU.mult, [PK[4], "consts"], ["attC"])
                TTo("dve", attA[:], ps[5][:, :].rearrange("p (h t) -> p h t", h=4), CF(4).unsqueeze(1).to_broadcast([128, 4, 128]),
                    ALU.mult, [PK[5], "consts"], ["attA"])
            b = 3
            MM(ps[b][:, :], CR(2), lgp[:, :], True, True, ["lgp", "consts"], [PK[b]])
            ACT(tmp512[:], ps[b][:, :], AF.Exp, [PK[b]], ["tmp512"])
            TTo("dve", kdec[:], k_tok[:], tmp512[:], ALU.mult, ["k_tok", "tmp512"], ["kdec"])
            for h in range(4):
                sk = "Sg%d" % h
                ob = 4 + h // 2
                oap = ps[ob][:, (h % 2) * 256:(h % 2) * 256 + 256]
                vh = v_tok[:, h * 256:(h + 1) * 256]
                if full:
                    MM(oap, attC[:, h, :], vh, True, False, ["attC", "v_tok"], [PK[ob]])
                    MM(oap, attA[:, h, :], vh, False, False, ["attA", "v_tok"], [PK[ob]])
                    MM(oap, qe0[:, h, :], Sg[:, h, :], False, False, ["qe0", sk], [PK[ob]])
                for c in range(2):
                    sb_ = 0 + c
                    MM(ps[sb_][:, 0:256], kdec[c * 64:(c + 1) * 64, h * 128:(h + 1) * 128], v_tok[c * 64:(c + 1) * 64, h * 256:(h + 1) * 256],
                       True, True, ["kdec", "v_tok"], [PK[sb_]])
                    STT("dve", Sg[:, h, :], Sg[:, h, :].bitcast(F32), eG[:, h, c * 64 + 63:c * 64 + 64], ps[sb_][:, 0:256], ALU.mult, ALU.add,
                        [sk, "eG", PK[sb_]], [sk])
                    if full and c == 0:
                        MM(oap, qe1[:, h, :], Sg[:, h, :], False, True, ["qe1", sk], [PK[ob]])
            if full:
                tm_proj("w_in", C_GO, 1024, lambda b, o, cw: ACT(sg[:, o:o + cw], ps[b][:, 0:cw], AF.Silu, [PK[b]], ["sg"]))
                for h in range(4):
                    ob = 4 + h // 2
                    oap = ps[ob][:, (h % 2) * 256:(h % 2) * 256 + 256]
                    S.op("act", lambda e, oap=oap, h=h: e.activation(out=tmp512[:, 0:256], in_=oap, func=AF.Square,
                                                                    accum_out=st8[:, h:h + 1]), [PK[ob]], ["st8", "tmp512"])
                rstd_from(st8[:, 4:8], st8[:, 0:4], 256.0, ["st8"], ["st8"])
                for hh in range(2):
                    TTo("dve", o_sb[:, hh * 512:(hh + 1) * 512].rearrange("p (h v) -> p h v", h=2),
                        ps[4 + hh][:, :].rearrange("p (h v) -> p h v", h=2),
                        st8[:, 4 + 2 * hh:6 + 2 * hh].unsqueeze(2).to_broadcast([128, 2, 256]), ALU.mult, [PK[4 + hh], "st8"], ["o_sb"])
                TTo(PL, o_sb[:], o_sb[:], sg[:], ALU.mult, ["o_sb", "sg"], ["o_sb"])
                for hh in range(2):
                    b = 2 + hh
                    for j in range(4):
                        cch = hh * 4 + j
                        TR(ps[b][:, j * 128:(j + 1) * 128], o_sb[:, cch * 128:(cch + 1) * 128], ["o_sb"], [PK[b]])
                    for j in range(4):
                        cch = hh * 4 + j
                        S.op("act", lambda e, b=b, j=j, cch=cch: e.mul(out=oT[:, cch, :], in_=ps[b][:, j * 128:(j + 1) * 128], mul=gnw_fm[:, cch % 2:cch % 2 + 1]),
                             [PK[b], "smallA"], ["oT"])

            b = 3
            for j, ci in enumerate((3, 4, 5, 6)):
                MM(ps[b][:, j * 32:(j + 1) * 32], CR(ci), a_sb[:, :], True, True, ["consts", "a_sb"], [PK[b]])
            ACT(f2(ex4[:]), ps[b][:, 0:128], AF.Exp, [PK[b]], ["ex4"])
            for hp in range(2):
                hs = slice(16 * hp, 16 * hp + 16)
                xw = xbc[:, 12 * hp:12 * hp + 12, :]
                for i in range(4):
                    win = xw[:, :, i:i + 128]
                    wbc = convw[:, 12 * hp:12 * hp + 12, i:i + 1].to_broadcast([128, 12, 128])
                    if i == 0:
                        TTo("dve", acc[:], win, wbc, ALU.mult, ["xbc", "smallA"], ["acc"])
                    else:
                        eng = PL if i % 2 else "dve"
                        TTo(eng, acc2[:], win, wbc, ALU.mult, ["xbc", "smallA"], ["acc2"])
                        TTo(eng, acc[:], acc[:], acc2[:], ALU.add, ["acc", "acc2"], ["acc"])
                TTo("dve", acc[:], acc[:], convb[:, 12 * hp:12 * hp + 12].unsqueeze(2).to_broadcast([128, 12, 128]), ALU.add, ["acc", "smallA"], ["acc"])
                if hp == 1:
                    CP(PL, xbc[:, :, 0:3], xbc[:, :, 128:131], ["xbc"], ["xbc"])
                ACT(f2(xsT[:]), f2(acc[:, 0:8, :]), AF.Silu, ["acc"], ["xsT"])
                ACT(f2(bcT[:]), f2(acc[:, 8:12, :]), AF.Silu, ["acc"], ["bcT"])
                for q2 in range(2):
                    b = 4 + q2
                    for j in range(4):
                        TR(ps[b][:, j * 128:(j + 1) * 128], xsT[:, q2 * 4 + j, :], ["xsT"], [PK[b]])
                    TTo("dve", xdt[:, q2 * 512:(q2 + 1) * 512].rearrange("p (h v) -> p h v", h=8),
                        ps[b][:, :].rearrange("p (h v) -> p h v", h=8),
                        dt_sb[:, 16 * hp + q2 * 8:16 * hp + q2 * 8 + 8].unsqueeze(2).to_broadcast([128, 8, 64]), ALU.mult, [PK[b], "dt_sb"], ["xdt"])
                    if full:
                        CP("act", xs_tok[:, q2 * 512:(q2 + 1) * 512], ps[b][:, :], [PK[b]], ["xs_tok"])
                b = 2
                for j in range(2):
                    TR(ps[b][:, j * 128:(j + 1) * 128], bcT[:, j, :].bitcast(F32), ["bcT"], [PK[b]])
                CP("act", B_tok[:], ps[b][:, 0:256], [PK[b]], ["B_tok"])
                expA = ex4[:, 0, hs]; toend = ex4[:, 1, hs]
                TTo("dve", xdte[:].rearrange("p (h v) -> p h v", h=16), xdt[:].bitcast(F32).rearrange("p (h v) -> p h v", h=16),
                    toend.unsqueeze(2).to_broadcast([128, 16, 64]), ALU.mult, ["xdt", "ex4"], ["xdte"])
                if full:
                    TTo("dve", R1[:], a_sb[:, hs].bitcast(F32).unsqueeze(2).to_broadcast([128, 16, 64]),
                        triL.unsqueeze(1).to_broadcast([128, 16, 64]), ALU.mult, ["a_sb", "consts"], ["R1"])
                    CP("dve", R2[:], a_sb[:, hs].bitcast(F32).unsqueeze(2).to_broadcast([128, 16, 64]), ["a_sb"], ["R2"])
                    for gl in range(2):
                        MM(ps[3][:, gl * 128:(gl + 1) * 128], bcT[:, gl, :], bcT[:, 2 + gl, :], True, True, ["bcT"], [PK[3]])
                    for gl in range(2):
                        b = 0 + gl
                        MM(ps[b][:, :], bd_ones[:], f2(R1[:, 8 * gl:8 * gl + 8, :]), True, False, ["bd", "R1"], [PK[b]])
                        MM(ps[b][:, :], negC[:], f2(R2[:, 8 * gl:8 * gl + 8, :]), False, True, ["bd", "R2"], [PK[b]])
                        ACT(seg[:, gl * 512:(gl + 1) * 512], ps[b][:, :], AF.Abs, [PK[b]], ["seg"])
                    ACT(seg[:], seg[:], AF.Exp, ["seg"], ["seg"], scale=-1.0)
                    for c in range(2):
                        pslc = slice(c * 64, (c + 1) * 64)
                        TTo("dve", MT[pslc, :, c * 64:(c + 1) * 64].rearrange("p (g r) t -> p g r t", g=2),
                            seg[pslc, :].rearrange("p (g r t) -> p g r t", g=2, r=8),
                            ps[3][pslc, 0:256].rearrange("p (g t) -> p g t", g=2)[:, :, c * 64:(c + 1) * 64].unsqueeze(2).to_broadcast([64, 2, 8, 64]),
                            ALU.mult, ["seg", PK[3]], ["MT"])
                    TTo("dve", Cm0[:], bcT[:, 2:4, :].bitcast(F32), CF(7).unsqueeze(1).to_broadcast([128, 2, 128]), ALU.mult, ["bcT", "consts"], ["Cm0"])
                    TTo("dve", Cm1[:], bcT[:, 2:4, :].bitcast(F32), CF(8).unsqueeze(1).to_broadcast([128, 2, 128]), ALU.mult, ["bcT", "consts"], ["Cm1"])
                for gl in range(2):
                    g = 2 * hp + gl
                    sk = "ST%d" % g
                    STg = ST[:, g * 512:(g + 1) * 512]
                    yb = 2
                    if full:
                        MM(ps[yb][:, :], Cm0[:, gl, :], STg, True, False, ["Cm0", sk], [PK[yb]])
                    for c in range(2):
                        sb_ = 0 + c
                        MM(ps[sb_][:, :], B_tok[c * 64:(c + 1) * 64, gl * 128:(gl + 1) * 128], xdte[c * 64:(c + 1) * 64, gl * 512:(gl + 1) * 512],
                           True, True, ["B_tok", "xdte"], [PK[sb_]])
                        TTo("dve", STg.rearrange("p (h v) -> p h v", h=8), STg.bitcast(F32).rearrange("p (h v) -> p h v", h=8),
                            ex4[:, 2 + c, g * 8:(g + 1) * 8].unsqueeze(2).to_broadcast([128, 8, 64]), ALU.mult, [sk, "ex4"], [sk])
                        TTo("dve", STg, STg.bitcast(F32), ps[sb_][:, :], ALU.add, [sk, PK[sb_]], [sk])
                        if full and c == 0:
                            MM(ps[yb][:, :], Cm1[:, gl, :], STg, False, True, ["Cm1", sk], [PK[yb]])
                    if full:
                        TTo("dve", yi[:, gl * 512:(gl + 1) * 512].rearrange("p (h v) -> p h v", h=8),
                            ps[yb][:, :].rearrange("p (h v) -> p h v", h=8),
                            expA[:, gl * 8:(gl + 1) * 8].unsqueeze(2).to_broadcast([128, 8, 64]), ALU.mult, [PK[yb], "ex4"], ["yi"])
                if not full:
                    continue
                for hl in range(16):
                    b = 4 + hl // 8
                    MM(ps[b][:, (hl % 8) * 64:(hl % 8) * 64 + 64], MT[:, hl, :], xdt[:, hl * 64:(hl + 1) * 64], True, True, ["MT", "xdt"], [PK[b]])
                for gl in range(2):
                    TTo("dve", y_sb[:, gl * 512:(gl + 1) * 512], ps[4 + gl][:, :], yi[:, gl * 512:(gl + 1) * 512], ALU.add, [PK[4 + gl], "yi"], ["y_sb"])
                TTo(PL, t2[:].rearrange("p (h v) -> p h v", h=16), xs_tok[:].rearrange("p (h v) -> p h v", h=16),
                    dsk_bc[:, hs].unsqueeze(2).to_broadcast([128, 16, 64]), ALU.mult, ["xs_tok", "smallB"], ["t2"])
                TTo(PL, y_sb[:], y_sb[:], t2[:], ALU.add, ["y_sb", "t2"], ["y_sb"])
                tm_proj("w_in", C_Z + hp * 1024, 1024, lambda b, o, cw: ACT(sz[:, o:o + cw], ps[b][:, 0:cw], AF.Silu, [PK[b]], ["sz"]))
                TTo("dve", y_sb[:], y_sb[:], sz[:], ALU.mult, ["y_sb", "sz"], ["y_sb"])
                for gl in range(2):
                    S.op("act", lambda e, gl=gl: e.activation(out=t2[:, 0:512], in_=y_sb[:, gl * 512:(gl + 1) * 512], func=AF.Square,
                                                             accum_out=st8[:, 8 + gl:9 + gl]), ["y_sb"], ["st8", "t2"])
                rstd_from(st8[:, 12:14], st8[:, 8:10], 512.0, ["st8"], ["st8"])
                TTo("dve", y_sb[:].rearrange("p (g v) -> p g v", g=2), y_sb[:].rearrange("p (g v) -> p g v", g=2),
                    st8[:, 12:14].unsqueeze(2).to_broadcast([128, 2, 512]), ALU.mult, ["y_sb", "st8"], ["y_sb"])
                for q2 in range(2):
                    b = 4 + q2
                    for j in range(4):
                        TR(ps[b][:, j * 128:(j + 1) * 128], y_sb[:, (q2 * 4 + j) * 128:(q2 * 4 + j + 1) * 128], ["y_sb"], [PK[b]])
                    for j in range(4):
                        cch = 8 * hp + q2 * 4 + j
                        S.op("act", lambda e, b=b, j=j, cch=cch: e.mul(out=yT[:, cch, :], in_=ps[b][:, j * 128:(j + 1) * 128], mul=snw_fm[:, cch:cch + 1]),
                             [PK[b], "smallA"], ["yT"])
            if not full:
                return
            for half in range(2):
                fm_proj("w_in", C_GA + half * 512, 512, hnT, "hnT",
                        lambda b, o, cw: ACT(f2(ga[:, o // 128:o // 128 + cw // 128, :]), ps[b][:, 0:cw], AF.Sigmoid, [PK[b]], ["ga"]))
                fm_proj("w_in", C_GB + half * 512, 512, hnT, "hnT",
                        lambda b, o, cw: ACT(f2(gb[:, o // 128:o // 128 + cw // 128, :]), ps[b][:, 0:cw], AF.Sigmoid, [PK[b]], ["gb"]))
                fm_proj("w_up_gla", half * 512, 512, oT, "oT",
                        lambda b, o, cw: TTo("dve", m1[:, o:o + cw], ps[b][:, 0:cw], f2(ga[:, o // 128:o // 128 + cw // 128, :]), ALU.mult, [PK[b], "ga"], ["m1"]))
                fm_proj("w_up_ssd", half * 512, 512, yT, "yT",
                        lambda b, o, cw: TTo("dve", m2[:, o:o + cw], ps[b][:, 0:cw], f2(gb[:, o // 128:o // 128 + cw // 128, :]), ALU.mult, [PK[b], "gb"], ["m2"]))
                TTo("dve", f2(mixT[:, half * 4:half * 4 + 4, :]), m1[:], m2[:], ALU.add, ["m1", "m2"], ["mixT"])
            fm_proj("w_out", 0, 1024, mixT, "mixT",
                    lambda b, o, cw: TTo("dve", f2(h1T[:, o // 128:o // 128 + cw // 128, :]), ps[b][:, 0:cw], f2(xt[:, o // 128:o // 128 + cw // 128, :]), ALU.add,
                                         [PK[b], xkey], ["h1T"]))
            if next_x is not None:
                load_x(*next_x)

        gsel = [0]

        def gather(table, col, eidx, kei):
            i = gsel[0] % NSLOT
            gsel[0] += 1
            key = "gs%d" % i
            sl = slots[i]
            S.dma("pool", lambda e: e.indirect_dma_start(out=sl[:, :], out_offset=None, in_=table[:, :],
                                                         in_offset=bass.IndirectOffsetOnAxis(ap=eidx[:, col:col + 1], axis=0)),
                  key, reads=[kei, "uvbf"], writes=[key])
            return sl, key

        def peer_prologue(ti):
            g = ti % 2
            h1tok, hn2tok, eidx, wsm = h1toks[g], hn2toks[g], eidxs[g], wsms[g]
            kh1, khn, kei, kws = "h1tok%d" % g, "hn2tok%d" % g, "eidx%d" % g, "wsm%d" % g
            norm_fm(h1T[:], "h1T", lnffn, hnT, "hnT", f32copy=True)
            fm_proj("peer_wq", 0, 2048, hnT, "hnT",
                    lambda b, o, cw: CP("act" if (o // 512) % 2 else "dve", f2(pqT[:, o // 128:o // 128 + cw // 128, :]), ps[b][:, 0:cw], [PK[b]], ["pqT"]))
            for blk in range(4):
                b = 4 + (blk % 2)
                for j in range(4):
                    c = blk * 4 + j
                    MM(ps[b][:, j * 128:(j + 1) * 128], pqT[:, c, :], skT[:, c, :], True, True, ["pqT", "skT"], [PK[b]])
                CP("act", f2(sc[:, blk * 4:blk * 4 + 4, :]), ps[b][:, :], [PK[b]], ["sc"])
            for hh in range(2):
                b = 2 + hh
                for j in range(4):
                    TR(ps[b][:, j * 128:(j + 1) * 128], h1T[:, hh * 4 + j, :], ["h1T"], [PK[b]])
                CP("act", h1tok[:, hh * 512:(hh + 1) * 512], ps[b][:, :], [PK[b]], [kh1])
            for hh in range(2):
                b = 2 + hh
                for j in range(4):
                    TR(ps[b][:, j * 128:(j + 1) * 128], sq[:, hh * 4 + j, :], ["sq"], [PK[b]])
                CP("act", hn2tok[:, hh * 512:(hh + 1) * 512], ps[b][:, :], [PK[b]], [khn])
            for c in range(16):
                S.op("dve", lambda e, c=c: e.max(out=stv[:, c, 0:8], in_=sc[:, c, :]), ["sc"], ["stv"])
                S.op("dve", lambda e, c=c: e.match_replace(out=work[:, 0:128], in_to_replace=stv[:, c, 0:8], in_values=sc[:, c, :], imm_value=-1e30),
                     ["sc", "stv"], ["work"])
                S.op("dve", lambda e, c=c: e.max(out=stv[:, c, 8:16], in_=work[:, 0:128]), ["work"], ["stv"])
                S.op("dve", lambda e, c=c: e.max_index(out=sti[:, c, 0:8], in_max=stv[:, c, 0:8], in_values=sc[:, c, :]), ["sc", "stv"], ["sti"])
                S.op("dve", lambda e, c=c: e.max_index(out=sti[:, c, 8:16], in_max=stv[:, c, 8:16], in_values=sc[:, c, :]), ["sc", "stv"], ["sti"])
            CP("dve", sif[:], sti[:], ["sti"], ["sif"])
            stv4 = stv.rearrange("p (h i) k -> p h i k", i=2)
            sif4 = sif.rearrange("p (h i) k -> p h i k", i=2)
            TTo("dve", cand.rearrange("p h (a b) -> p h a b", a=16),
                stv4[:, :, 0, :].unsqueeze(3).to_broadcast([128, 8, 16, 16]),
                stv4[:, :, 1, :].unsqueeze(2).to_broadcast([128, 8, 16, 16]), ALU.add, ["stv"], ["cand"])
            for h in range(8):
                S.op("dve", lambda e, h=h: e.max(out=best[:, h, 0:8], in_=cand[:, h, :]), ["cand"], ["best"])
                S.op("dve", lambda e, h=h: e.match_replace(out=work[:, :], in_to_replace=best[:, h, 0:8], in_values=cand[:, h, :], imm_value=-1e30),
                     ["cand", "best"], ["work"])
                S.op("dve", lambda e, h=h: e.max(out=best[:, h, 8:16], in_=work[:, :]), ["work"], ["best"])
                S.op("dve", lambda e, h=h: e.max_index(out=pos[:, h, 0:8], in_max=best[:, h, 0:8], in_values=cand[:, h, :]), ["cand", "best"], ["pos"])
                S.op("dve", lambda e, h=h: e.max_index(out=pos[:, h, 8:16], in_max=best[:, h, 8:16], in_values=cand[:, h, :]), ["cand", "best"], ["pos"])
            S.op("dve", lambda e: e.tensor_single_scalar(out=posa[:], in_=pos[:], scalar=4, op=ALU.logical_shift_right), ["pos"], ["posa"])
            S.op("dve", lambda e: e.tensor_single_scalar(out=posb[:], in_=pos[:], scalar=15, op=ALU.bitwise_and), ["pos"], ["posb"])
            CP("dve", af[:], posa[:], ["posa"], ["af"])
            CP("dve", bf[:], posb[:], ["posb"], ["bf"])
            io4 = iota16.unsqueeze(1).unsqueeze(1).to_broadcast([128, 8, 16, 16])
            for (srcf, half, dst, dk_) in ((af, 0, i0s, "i0s"), (bf, 1, i1s, "i1s")):
                TTo("dve", oh[:], srcf.unsqueeze(3).to_broadcast([128, 8, 16, 16]), io4, ALU.is_equal, ["af", "bf", "consts"], ["oh"])
                TTo("dve", oh[:], oh[:], sif4[:, :, half, :].unsqueeze(2).to_broadcast([128, 8, 16, 16]), ALU.mult, ["oh", "sif"], ["oh"])
                S.op("dve", lambda e, dst=dst: e.reduce_sum(out=dst, in_=oh, axis=AX.X), ["oh"], [dk_])
            STT("dve", eidx_f[:], f2(i0s), 128.0, f2(i1s), ALU.mult, ALU.add, ["i0s", "i1s"], ["eidx_f"])
            CP("dve", eidx[:], eidx_f[:], ["eidx_f"], [kei])
            TTo("dve", wsm[:], best[:], best[:, :, 0:1].to_broadcast([128, 8, 16]), ALU.subtract, ["best"], [kws])
            ACT(f2(wsm), f2(wsm), AF.Exp, [kws], [kws])
            S.op("dve", lambda e: e.reduce_sum(out=ssum, in_=wsm, axis=AX.X), [kws], ["ssum"])
            S.op("dve", lambda e: e.reciprocal(out=ssum, in_=ssum), ["ssum"], ["ssum"])
            TTo("dve", wsm[:], wsm[:], ssum.unsqueeze(2).to_broadcast([128, 8, 16]), ALU.mult, [kws, "ssum"], [kws])

        def peer_gather(ti):
            g = ti % 2
            h1tok, hn2tok, eidx, wsm = h1toks[g], hn2toks[g], eidxs[g], wsms[g]
            kh1, khn, kei, kws = "h1tok%d" % g, "hn2tok%d" % g, "eidx%d" % g, "wsm%d" % g
            S.op("dve", lambda e: e.memset(pre, 0.0), [], ["pre"])
            for j in range(128):
                sl, key = gather(u_bf, j, eidx, kei)
                STT("dve", sl[:], sl[:], 1.0, hn2tok[:], ALU.mult, ALU.mult, [key, khn], ["pre", key], accum=pre[:, j:j + 1])
                yield
            TTo("dve", gtmp[:], pre[:], pre[:], ALU.mult, ["pre"], ["gtmp"])
            TS("dve", gtmp[:], gtmp[:], 0.044715, ALU.mult, ["gtmp"], ["gtmp"], s2=1.0, op1=ALU.add)
            TTo("dve", gtmp[:], gtmp[:], pre[:], ALU.mult, ["gtmp", "pre"], ["gtmp"])
            ACT(gtmp[:], gtmp[:], AF.Sigmoid, ["gtmp"], ["gtmp"], scale=1.5957691216057308)
            TTo("dve", gtmp[:], gtmp[:], pre[:], ALU.mult, ["gtmp", "pre"], ["gtmp"])
            TTo("dve", coef[:], gtmp[:], f2(wsm), ALU.mult, ["gtmp", kws], ["coef"])
            yield
            for j in range(128):
                sl, key = gather(v_bf, j, eidx, kei)
                dg = dgs[j % NDG]; dk_ = "dg%d" % (j % NDG)
                TS("dve", dg[:], ident, coef[:, j:j + 1], ALU.mult, ["coef", "consts"], [dk_])
                MM(ps[6][:, :], dg[:], sl[:, 0:512], j == 0, j == 127, [dk_, key], [PK[6]])
                MM(ps[7][:, :], dg[:], sl[:, 512:1024], j == 0, j == 127, [dk_, key], [PK[7]])
                yield
            TTo("dve", h1tok[:, 0:512], h1tok[:, 0:512], ps[6][:, :], ALU.add, [kh1, PK[6]], [kh1])
            TTo("dve", h1tok[:, 512:1024], h1tok[:, 512:1024], ps[7][:, :], ALU.add, [kh1, PK[7]], [kh1])
            S.op("act", lambda e: e.activation(out=hn2tok[:], in_=h1tok[:], func=AF.Square, accum_out=st9[:, 0:1]), [kh1], ["st9", khn])
            rstd_from(st9[:, 1:2], st9[:, 0:1], 1024.0, ["st9"], ["st9"])
            STT("dve", h1tok[:], h1tok[:], st9[:, 1:2], lnf_bc, ALU.mult, ALU.mult, [kh1, "st9", "smallB"], [kh1])
            S.dma("sp", lambda e: e.dma_start(out=out_d[ti * 128:(ti + 1) * 128, :], in_=h1tok[:]), "ostore%d" % g,
                  reads=[kh1], writes=["o_out%d" % g])
            yield

        S.pump = conv_tables()
        S.pump_rate = max(1, (NPT * 600) // 100)
        prefix_mode[0] = True
        for ti in range(NPT):
            mixer_tile(xpT, ti, False, maskp[:, ti:ti + 1], 0, last_prefix=(ti == NPT - 1), xloaded=(ti > 0),
                       next_x=((xpT, ti + 1) if ti + 1 < NPT else (xmT, 0)))
        prefix_mode[0] = False
        S.drain()
        S.seal("cv1", ["wscr1"])
        S.seal("cvt", ["uvbf"])
        S.pump_rate = 4
        def main_tile(ti):
            mixer_tile(xmT, ti, True, None, 0, xloaded=True, next_x=((xmT, ti + 1) if ti + 1 < NMT else None))
            if dbg:
                dv = dbg_h1T.rearrange("(k p) t -> p k t", p=128)[:, :, ti * 128:(ti + 1) * 128]
                S.dma("sp", lambda e, dv=dv: e.dma_start(out=dv, in_=h1T[:]), "dbg", reads=["h1T"], writes=["dbg_out"])
            if dbg != "mixer":
                peer_prologue(ti)
        main_tile(0)
        for ti in range(NMT):
            if dbg != "mixer":
                S.pump = peer_gather(ti)
                S._pc = 0
                S.pump_rate = 5
            if ti + 1 < NMT:
                main_tile(ti + 1)
            S.drain()
        fin = (["o_out0", "o_out1"] if dbg != "mixer" else []) + (["dbg_out"] if dbg else [])
        S.wait_all("sp", fin)
        S.limit = limit
        build.last_sched = S
        S.emit()
    return nc


def pack_shared(inp):
    f = lambda a: np.ascontiguousarray(np.asarray(a, dtype=np.float32))
    sh = {}
    sh["w_in"] = f(inp["w_in"][0])
    sh["w2aug"] = f(np.concatenate([inp["gla_w_gate2"][0], inp["gla_b_gate"][0][None, :]], axis=0))
    sh["w_up_gla"] = f(inp["w_up_gla"][0]); sh["w_up_ssd"] = f(inp["w_up_ssd"][0]); sh["w_out"] = f(inp["w_out"][0])
    sh["peer_wq"] = f(inp["peer_w_q"][0])
    sk = np.asarray(inp["peer_sub_keys"][0], np.float32)
    sh["skT"] = f(sk.reshape(16, 128, 128).transpose(2, 0, 1).reshape(128, 16 * 128))
    sh["peer_u"] = f(inp["peer_u"][0]); sh["peer_v"] = f(inp["peer_v"][0])
    sh["consts"] = make_consts()
    fm = lambda v: np.asarray(v, np.float32).reshape(-1, 128).T
    cw = np.asarray(inp["ssd_conv_w"][0], np.float32)
    convw = cw.reshape(4, 24, 128).transpose(2, 1, 0)[:, XPERM, :].reshape(128, 96)
    convb = fm(inp["ssd_conv_b"][0])[:, XPERM]
    sh["smallA"] = f(np.concatenate([fm(inp["ln_mix_w"][0]), fm(inp["ln_ffn_w"][0]), convw, convb,
                                     fm(inp["gla_norm_w"][0]), fm(inp["ssd_norm_w"][0])], axis=1))
    bc = lambda v: np.broadcast_to(np.asarray(v, np.float32).reshape(1, -1), (128, np.asarray(v).size))
    sh["smallB"] = f(np.concatenate([bc(inp["ln_final_w"]), bc(inp["ssd_dt_bias"][0]), bc(inp["ssd_a_log"][0]), bc(inp["ssd_d"][0])], axis=1))
    return sh


def core_inputs(x_b, meta, half, NM, NP):
    chunk0 = np.concatenate([np.zeros((48, D), np.float32), meta], axis=0)
    m0 = np.concatenate([np.zeros(48, np.float32), np.ones(16, np.float32)])
    if half == 0:
        pre = np.concatenate([np.zeros((NP - 64, D), np.float32), chunk0], axis=0)
        mk = np.concatenate([np.zeros(NP - 64, np.float32), m0])
        main = x_b[0:NM]
    else:
        pre = np.concatenate([np.zeros((64, D), np.float32), chunk0, x_b[0:NM]], axis=0)
        mk = np.concatenate([np.zeros(64, np.float32), m0, np.ones(NM, np.float32)])
        main = x_b[NM:2 * NM]
    assert pre.shape[0] == NP
    return {"xpT": np.ascontiguousarray(pre.T), "xmT": np.ascontiguousarray(main.T),
            "maskp": np.ascontiguousarray(mk.reshape(NP // 128, 128).T)}


_NC_CACHE = {}


def kernel(**inputs):
    x = np.asarray(inputs["x"], np.float32)
    B, L, _ = x.shape
    NM = L // 2
    NMT = NM // 128
    NPT = NMT + 1
    meta = np.asarray(inputs["meta_tokens"], np.float32)
    sh = pack_shared(inputs)
    in_maps = []
    for b in range(B):
        for half in range(2):
            m = dict(sh)
            m.update(core_inputs(x[b], meta, half, NM, NPT * 128))
            in_maps.append(m)
    key = (NPT, NMT)
    if key not in _NC_CACHE:
        _NC_CACHE[key] = build(NPT, NMT)
    nc = _NC_CACHE[key]
    res = run_bass_kernel_spmd(nc, in_maps, core_ids=list(range(len(in_maps))))
    out = np.zeros((B, L, D), np.float32)
    for b in range(B):
        for half in range(2):
            out[b, half * NM:(half + 1) * NM] = res.results[b * 2 + half]["out"]
    return out
```

```python
import contextlib
import numpy as np
import concourse.bass as bass
import concourse.mybir as mybir
from concourse.bass_utils import run_bass_kernel_spmd

F32 = mybir.dt.float32
F32R = mybir.dt.float32
I32 = mybir.dt.int32
BF16 = mybir.dt.bfloat16
U32 = mybir.dt.uint32
AF = mybir.ActivationFunctionType
ALU = mybir.AluOpType
AX = mybir.AxisListType

ENGS = ("pe", "act", "dve", "pool", "sp")


class Sched:
    def __init__(self, nc):
        self.nc = nc
        self.ops = {e: [] for e in ENGS}
        self.count = {e: 0 for e in ENGS}
        self.known = {e: {} for e in ENGS}
        self.last_write = {}
        self.readers = {}
        self.dma_cnt = {}
        self.overlaps = {}
        self.semkeys = set()
        self.seq = 0
        self.labels = []
        self.limit = None
        self.pump = None
        self.pump_rate = 4
        self._pc = 0
        self._inpump = False
    EPOCH = 3000
    DEPOCH = 187

    def _deps(self, eng, reads, writes):
        deps = {}

        def add(d, same_ok):
            if d is None:
                return
            s, v = d
            if s[0] == eng and not same_ok:
                return
            if deps.get(s, 0) < v:
                deps[s] = v
        for r in reads:
            add(self.last_write.get(r), eng != "pe")
        for w in writes:
            for k in [w] + self.overlaps.get(w, []):
                add(self.last_write.get(k), eng != "pe")
                for d in self.readers.get(k, ()):
                    add(d, eng != "pe")
        waits = []
        kn = self.known[eng]
        for s, v in deps.items():
            if kn.get(s, 0) < v:
                kn[s] = v
                waits.append((s, v))
        return waits

    def _commit(self, tag, reads, writes):
        for r in reads:
            self.readers.setdefault(r, []).append(tag)
        for w in writes:
            self.last_write[w] = tag
            self.readers[w] = []

    @staticmethod
    def _psx(reads, writes):
        pr = [r for r in reads if r.startswith("ps")]
        if not pr:
            return list(reads), list(writes)
        return [r for r in reads if not r.startswith("ps")], list(writes) + [r for r in pr if r not in writes]

    def _maybe_pump(self):
        if self.pump is None or self._inpump:
            return
        self._pc += 1
        if self._pc % self.pump_rate:
            return
        self._inpump = True
        try:
            next(self.pump)
        except StopIteration:
            self.pump = None
        self._inpump = False

    def drain(self):
        if self.pump is None:
            return
        self._inpump = True
        for _ in self.pump:
            pass
        self._inpump = False
        self.pump = None

    def op(self, eng, fn, reads=(), writes=()):
        self._maybe_pump()
        reads, writes = self._psx(reads, writes)
        waits = self._deps(eng, reads, writes)
        self.count[eng] += 1
        idx = self.count[eng]
        tag = ((eng,), idx)
        self.ops[eng].append((fn, waits, ("eng", eng, idx), self.seq))
        self._note(eng)
        self._commit(tag, reads, writes)

    def dma(self, q, fn, semkey, reads=(), writes=()):
        self._maybe_pump()
        waits = self._deps(q, reads, writes)
        self.dma_cnt[semkey] = self.dma_cnt.get(semkey, 0) + 1
        c = self.dma_cnt[semkey] - 1
        sk = ("dma", semkey, c // self.DEPOCH)
        tag = (sk, 16 * (c % self.DEPOCH + 1))
        self.semkeys.add(sk)
        self.ops[q].append((fn, waits, (sk, 16), self.seq))
        self._note(q + "-dma")
        self._commit(tag, reads, writes)

    def seal(self, semkey, keys):
        sk = ("dma", semkey, 0)
        for k in keys:
            self.last_write[k] = (sk, 16 * self.dma_cnt[semkey])

    def wait_all(self, eng, keys):
        waits = self._deps(eng, list(keys), ())
        self.ops[eng].append((None, waits, None, self.seq))
        self._note(eng + "-waitall")

    def _note(self, what):
        import sys as _s
        f = _s._getframe(2)
        ln = []
        for _ in range(4):
            if f is None:
                break
            ln.append(f.f_lineno)
            f = f.f_back
        self.labels.append((what, ln))
        self.seq += 1

    def emit(self):
        nc = self.nc
        waited = {e: set() for e in ENGS}
        for e in ENGS:
            for fn, waits, inc, seq in self.ops[e]:
                for sk, v in waits:
                    if len(sk) == 1:
                        waited[sk[0]].add(v)
        sig = {}
        semkeys = set(self.semkeys)
        for e in ENGS:
            c = 0
            for fn, waits, inc, seq in self.ops[e]:
                if inc is not None and inc[0] == "eng" and inc[2] in waited[e]:
                    sig[(e, inc[2])] = ((e, c // self.EPOCH), c % self.EPOCH + 1)
                    semkeys.add((e, c // self.EPOCH))
                    c += 1
        self.n_signals = len(sig)

        def tr(sk, v):
            return sig[(sk[0], v)] if len(sk) == 1 else (sk, v)
        with contextlib.ExitStack() as st:
            sems = {}
            for i, k in enumerate(sorted(semkeys, key=str)):
                sems[k] = st.enter_context(nc.semaphore("s%d" % i))
            block = st.enter_context(nc.Block())
            engmap = {"pe": block.tensor, "act": block.scalar, "dve": block.vector,
                      "pool": block.gpsimd, "sp": block.sync}
            for e in ENGS:
                ops = self.ops[e]
                if not ops:
                    continue

                def body(eng, ops=ops):
                    for fn, waits, inc, seq in ops:
                        if self.limit is not None and seq >= self.limit:
                            break
                        for s, v in waits:
                            s2, v2 = tr(s, v)
                            eng.wait_ge(sems[s2], v2)
                        if fn is not None:
                            ins = fn(eng)
                            if inc is not None:
                                if inc[0] == "eng":
                                    sg_ = sig.get((inc[1], inc[2]))
                                    if sg_ is not None:
                                        ins.then_inc(sems[sg_[0]], 1)
                                else:
                                    ins.then_inc(sems[inc[0]], inc[1])
                engmap[e](body)
        return nc


D = 1024
INW = 10288
C_Q, C_K, C_V, C_GO, C_GL, C_Z, C_XBC, C_DT, C_GA, C_GB = 0, 512, 1024, 2048, 3072, 3088, 5136, 8208, 8240, 9264
NCONST = 9
EPS = 1e-6


def make_consts():
    p = np.arange(128)[:, None]
    f = np.arange(128)[None, :]
    same = (p // 64) == (f // 64)
    c = np.zeros((128, NCONST + 1, 128), np.float32)
    c[:, 0] = (p == f)
    c[:, 1] = np.where(same & (p <= f), -1.0 / 16, 0.0)
    c[:, 2] = np.where(same & (p > f), -1.0 / 16, 0.0)
    c[:, 3] = (same & (p <= f))
    c[:, 4] = (same & (p > f))
    c[:, 5] = (p // 64 == 0) * np.ones((1, 128))
    c[:, 6] = (p // 64 == 1) * np.ones((1, 128))
    c[:, 7] = np.ones((128, 1)) * (f // 64 == 0)
    c[:, 8] = np.ones((128, 1)) * (f // 64 == 1)
    c[:, 9, 0:64] = ((p % 64) <= np.arange(64)[None, :])
    c[:, 9, 64:80] = np.arange(16)[None, :]
    return c.reshape(128, (NCONST + 1) * 128)


XPERM = list(range(0, 8)) + [16, 17, 20, 21] + list(range(8, 16)) + [18, 19, 22, 23]


def build(NPT, NMT, dbg=False, limit=None):
    nc = bass.Bass("TRN2", target_bir_lowering=False)
    NP, NM = NPT * 128, NMT * 128

    def din(name, shape, dt=F32):
        return nc.dram_tensor(name, shape, dt, kind="ExternalInput").ap()

    xpT = din("xpT", [D, NP]); xmT = din("xmT", [D, NM]); maskp_d = din("maskp", [128, NPT])
    w_in = din("w_in", [D, INW]); w2aug_d = din("w2aug", [17, 512])
    w_up_gla = din("w_up_gla", [1024, D]); w_up_ssd = din("w_up_ssd", [2048, D]); w_out = din("w_out", [D, D])
    peer_wq = din("peer_wq", [D, 2048]); skT_d = din("skT", [128, 16 * 128])
    peer_u = din("peer_u", [16384, D]); peer_v = din("peer_v", [16384, D])
    consts_d = din("consts", [128, (NCONST + 1) * 128])
    NSA = 8 + 8 + 96 + 24 + 2 + 16
    smallA_d = din("smallA", [128, NSA])
    smallB_d = din("smallB", [128, 1024 + 96])
    out_d = nc.dram_tensor("out", [NM, D], F32, kind="ExternalOutput").ap()
    WB = {}
    wlist = []

    def add_w(wname, wap, KC, segs):
        CWmax = 512 if KC == 8 else 256
        for (c0, n) in segs:
            for o in range(0, n, CWmax):
                cw = min(CWmax, n - o)
                WB[(wname, c0 + o)] = (len(wlist), KC, cw)
                wlist.append((wap, KC, c0 + o, cw))
    add_w("w_in", w_in, 8, [(C_Q, 512), (C_K, 512), (C_V, 1024), (C_GO, 1024), (C_GL, 16), (C_Z, 2048), (C_XBC, 3072), (C_DT, 32),
                            (C_GA, 1024), (C_GB, 1024)])
    add_w("w_up_gla", w_up_gla, 8, [(0, 1024)])
    add_w("w_up_ssd", w_up_ssd, 16, [(0, 1024)])
    add_w("w_out", w_out, 8, [(0, 1024)])
    add_w("peer_wq", peer_wq, 8, [(0, 2048)])
    wscr = nc.dram_tensor("wscr", [len(wlist), 128, 4096], BF16, kind="Internal").ap()
    u_bf = nc.dram_tensor("u_bf", [16384, D], BF16, kind="Internal").ap()
    v_bf = nc.dram_tensor("v_bf", [16384, D], BF16, kind="Internal").ap()
    if dbg:
        dbg_h1T = nc.dram_tensor("dbg_h1T", [D, NM], F32, kind="ExternalOutput").ap()

    with contextlib.ExitStack() as st:
        def T(name, shape, dt=F32):
            return st.enter_context(nc.sbuf_tensor("sb_" + name, shape, dt))
        ps = [st.enter_context(nc.psum_tensor("ps%d" % i, [128, 512], F32)) for i in range(8)]
        PK = ["ps%d" % i for i in range(8)]
        S = Sched(nc)

        consts = T("consts", [128, (NCONST + 1) * 128], F32R)
        cF = consts[:].bitcast(F32)

        def CR(i):
            return consts[:, i * 128:(i + 1) * 128]

        def CF(i):
            return cF[:, i * 128:(i + 1) * 128]
        ident = CF(0)
        triL = cF[:, 9 * 128:9 * 128 + 64]
        iota16 = cF[:, 9 * 128 + 64:9 * 128 + 80]
        smallA = T("smallA", [128, NSA]); smallB = T("smallB", [128, 1120])
        lnmix = smallA[:, 0:8]; lnffn = smallA[:, 8:16]
        convw = smallA[:, 16:112].rearrange("p (c i) -> p c i", i=4); convb = smallA[:, 112:136]
        gnw_fm = smallA[:, 136:138]; snw_fm = smallA[:, 138:154]
        lnf_bc = smallB[:, 0:1024]
        dtb_bc = smallB[:, 1024:1056]; alog_bc = smallB[:, 1056:1088]; dsk_bc = smallB[:, 1088:1120]
        maskp = T("maskp", [128, NPT]); w2aug = T("w2aug", [17, 512], F32R); skT = T("skT", [128, 16, 128], F32R)
        negA = T("negA", [128, 32]); ones = T("ones", [128, 128], F32R)
        bd_ones = T("bd_ones", [128, 128], F32R); negC = T("negC", [128, 128], F32R)

        S.dma("sp", lambda e: e.dma_start(out=smallA[:], in_=smallA_d), "setup", writes=["smallA"])
        S.dma("sp", lambda e: e.dma_start(out=smallB[:], in_=smallB_d), "setup", writes=["smallB"])
        S.dma("sp", lambda e: e.dma_start(out=maskp[:], in_=maskp_d), "setup", writes=["maskp"])
        S.dma("pool", lambda e: e.dma_start(out=consts[:], in_=consts_d), "setupc", writes=["consts"])
        S.dma("pool", lambda e: e.dma_start(out=w2aug[:], in_=w2aug_d), "setupc", writes=["w2aug"])
        S.dma("pool", lambda e: e.dma_start(out=skT[:].rearrange("p a b -> p (a b)"), in_=skT_d), "setupc", writes=["skT"])
        S.seal("setup", ["smallA", "smallB", "maskp"])
        S.seal("setupc", ["consts", "w2aug", "skT"])
        def _early(bi):
            wap, KC, c0, cw = wlist[bi]
            return wap is w_in and ((C_K <= c0 < C_GO) or (C_GL <= c0 < C_Z) or (C_XBC <= c0 < C_GA))
        wgrp = {bi: (0 if _early(bi) else 1) for bi in range(len(wlist))}

        def conv_weights(grp):
            for bi, (wap, KC, c0, cw) in enumerate(wlist):
                if wgrp[bi] != grp:
                    continue
                dst = wscr[bi][:, 0:KC * cw].rearrange("p (k c) -> p k c", k=KC)
                src = wap.rearrange("(k p) c -> p k c", p=128)[:, :, c0:c0 + cw]
                S.dma("pool", lambda e, dst=dst, src=src: e.dma_start(out=dst, in_=src), "cv%d" % grp, writes=["wscr%d" % grp])
                yield
        for _ in conv_weights(0):
            pass
        S.seal("cv0", ["wscr0"])
        CR_ = 512

        def conv_tables():
            for _ in conv_weights(1):
                yield
            for (src_t, dst_t) in ((peer_u, u_bf), (peer_v, v_bf)):
                for r0 in range(0, 16384, CR_):
                    S.dma("pool", lambda e, src_t=src_t, dst_t=dst_t, r0=r0: e.dma_start(out=dst_t[r0:r0 + CR_, :], in_=src_t[r0:r0 + CR_, :]),
                          "cvt", writes=["uvbf"])
                    yield

        Sg = T("Sg", [128, 4, 256], F32R)
        ST = T("ST", [128, 2048], F32R)
        xbc = T("xbc", [128, 24, 131])
        MT = T("MT", [128, 16, 128], F32R)
        glaug = T("glaug", [32, 128], F32R)
        S.op("dve", lambda e: e.memset(Sg[:], 0.0), writes=["Sg0", "Sg1", "Sg2", "Sg3"])
        S.op("dve", lambda e: e.memset(ST[:], 0.0), writes=["ST0", "ST1", "ST2", "ST3"])
        S.op("dve", lambda e: e.memset(xbc[:], 0.0), writes=["xbc"])
        S.op("dve", lambda e: e.memset(MT[:], 0.0), writes=["MT"])
        S.op("dve", lambda e: e.memset(glaug[:], 1.0), writes=["glaug"])
        S.op("dve", lambda e: e.memset(ones[:], 1.0), writes=["ones"])
        S.op("act", lambda e: e.activation(out=negA[:], in_=alog_bc, func=AF.Exp), reads=["smallB"], writes=["negA"])
        S.op("dve", lambda e: e.tensor_scalar(out=negA[:], in0=negA[:], scalar1=-1.0, scalar2=None, op0=ALU.mult),
             reads=["negA"], writes=["negA"])
        S.op("dve", lambda e: e.tensor_tensor(out=bd_ones[:], in0=CF(3), in1=CF(4), op=ALU.add), ["consts"], ["bd"])
        S.op("dve", lambda e: e.tensor_scalar(out=negC[:], in0=CF(3), scalar1=-1.0, scalar2=None, op0=ALU.mult), ["consts"], ["bd"])

        NWB = 3
        wbt = [T("wb%d" % i, [128, 4096], BF16) for i in range(NWB)]
        wsel = [0]
        xT = [T("xT0", [128, 8, 128])]
        hnT = T("hnT", [128, 8, 128], BF16)
        sq = T("sq", [128, 8, 128], F32R)
        h1T = T("h1T", [128, 8, 128])
        rstd_bc = T("rstd_bc", [128, 128])
        st8 = T("st8", [128, 16])
        dt_sb = T("dt_sb", [128, 32]); a_sb = T("a_sb", [128, 32], F32R); ex4 = T("ex4", [128, 4, 32])

        ARW = 20736
        AR = T("arena", [128, ARW])
        abufs = {}

        def A(name, off, shape, dt=F32):
            size = 1
            for d_ in shape[1:]:
                size *= d_
            if dt == BF16:
                size //= 2
            assert off + size <= ARW, (name, off, size)
            ap = AR[0:shape[0], off:off + size]
            if dt != F32:
                ap = ap.bitcast(dt)
            if len(shape) == 3:
                ap = ap.rearrange("p (a b) -> p a b", a=shape[1])
            elif len(shape) == 4:
                ap = ap.rearrange("p (a b c) -> p a b c", a=shape[1], b=shape[2])
            abufs[name] = (off, size)
            return ap
        qT = A("qT", 0, [128, 4, 128]); kT = A("kT", 512, [128, 4, 128]); k_tok = A("k_tok", 1024, [128, 512])
        v_tok = A("v_tok", 1536, [128, 1024], F32R); lgp = A("lgp", 2560, [128, 512], F32R)
        oT = A("oT", 3072, [128, 8, 128], BF16); yT = A("yT", 3584, [128, 16, 128], BF16); mixT = A("mixT", 4608, [128, 8, 128], BF16)
        X = 5120
        eG = A("eG", X, [128, 4, 128]); eGn = A("eGn", X + 512, [128, 4, 128])
        qe = A("qe", X + 1024, [128, 4, 128], F32R); qn = A("qn", X + 1536, [128, 4, 128], F32R)
        ke = A("ke", X + 2048, [128, 4, 128], F32R); kn = A("kn", X + 2560, [128, 4, 128], F32R)
        qe0 = A("qe0", X + 3072, [128, 4, 128], F32R); qe1 = A("qe1", X + 3584, [128, 4, 128], F32R)
        attC = A("attC", X + 4096, [128, 4, 128], F32R); attA = A("attA", X + 4608, [128, 4, 128], F32R)
        kdec = A("kdec", X + 5120, [128, 512], F32R); tmp512 = A("tmp512", X + 5632, [128, 512])
        acc = A("acc", X, [128, 12, 128]); acc2 = A("acc2", X + 1536, [128, 12, 128])
        R1 = A("R1", X, [128, 16, 64], F32R); R2 = A("R2", X + 1024, [128, 16, 64], F32R); seg = A("seg", X + 2048, [128, 1024])
        m1 = A("m1", X + 3072, [128, 512]); m2 = A("m2", X + 3584, [128, 512])
        ga = A("ga", X + 4096, [128, 4, 128]); gb = A("gb", X + 4608, [128, 4, 128])
        Y = 11264
        xsT = A("xsT", Y, [128, 8, 128]); bcT = A("bcT", Y + 1024, [128, 4, 128], F32R)
        xdt = A("xdt", Y + 1536, [128, 1024], F32R); xdte = A("xdte", Y + 2560, [128, 1024], F32R)
        xs_tok = A("xs_tok", Y + 3584, [128, 1024]); B_tok = A("B_tok", Y + 4608, [128, 256], F32R)
        Cm0 = A("Cm0", Y + 4864, [128, 2, 128], F32R); Cm1 = A("Cm1", Y + 5120, [128, 2, 128], F32R)
        yi = A("yi", Y + 5376, [128, 1024]); y_sb = A("y_sb", Y + 6400, [128, 1024]); t2 = A("t2", Y + 7424, [128, 1024])
        sz = A("sz", Y + 8448, [128, 1024])
        wb_extra = [A("wb2", Y + 5376, [128, 4096], BF16), A("wb3", Y + 7424, [128, 4096], BF16),
                    A("wb4", 3072, [128, 4096], BF16), A("wb5", X + 3072, [128, 4096], BF16)]
        prefix_mode = [False]
        o_sb = A("o_sb", Y, [128, 1024]); sg = A("sg", Y + 1024, [128, 1024])
        pqT = A("pqT", 0, [128, 16, 128], F32R); sc = A("sc", 2048, [128, 16, 128]); cand = A("cand", 4096, [128, 8, 256])
        oh = A("oh", 6144, [128, 8, 16, 16])
        po = [8192]

        def AS(name, words, shape, dt=F32):
            ap = A(name, po[0], shape, dt)
            po[0] += words
            return ap
        work = AS("work", 256, [128, 256]); stv = AS("stv", 256, [128, 16, 16]); sti = AS("sti", 256, [128, 16, 16], U32)
        sif = AS("sif", 256, [128, 16, 16]); best = AS("best", 128, [128, 8, 16]); pos = AS("pos", 128, [128, 8, 16], U32)
        posa = AS("posa", 128, [128, 8, 16], U32); posb = AS("posb", 128, [128, 8, 16], U32)
        af = AS("af", 128, [128, 8, 16]); bf = AS("bf", 128, [128, 8, 16])
        i0s = AS("i0s", 128, [128, 8, 16]); i1s = AS("i1s", 128, [128, 8, 16])
        eidx_f = AS("eidx_f", 128, [128, 128]); ssum = AS("ssum", 8, [128, 8])
        h1toks = [T("h1tok%d" % i, [128, 1024])[:] for i in range(2)]
        hn2toks = [T("hn2b%d" % i, [128, 1024], BF16)[:] for i in range(2)]
        NSLOT = 8
        slots = [T("gs%d" % i, [128, 1024], BF16)[:] for i in range(NSLOT)]
        NDG = 4
        dgs = [T("dg%d" % i, [128, 128], BF16)[:] for i in range(NDG)]
        eidxs = [T("eidx%d" % i, [128, 128], I32)[:] for i in range(2)]
        wsms = [T("wsm%d" % i, [128, 8, 16])[:] for i in range(2)]
        pre = T("pre", [128, 128])[:]; gtmp = T("gtmp", [128, 128])[:]; coef = T("coef", [128, 128])[:]; st9 = T("st9", [128, 4])[:]
        names = list(abufs)
        for n1 in names:
            o1, s1 = abufs[n1]
            S.overlaps[n1] = [n2 for n2 in names if n2 != n1 and abufs[n2][0] < o1 + s1 and o1 < abufs[n2][0] + abufs[n2][1]]

        def MM(out, lhsT, rhs, start, stop, r, w):
            S.op("pe", lambda e: e.matmul(out, lhsT=lhsT, rhs=rhs, start=start, stop=stop), r, w)

        def TR(out, in_, r, w):
            S.op("pe", lambda e: e.transpose(out, in_, ident), list(r) + ["consts"], w)

        def ACT(out, in_, func, r, w, bias=None, scale=None, accum=None):
            kw = {}
            if bias is not None:
                kw["bias"] = bias
            if scale is not None:
                kw["scale"] = scale
            if accum is not None:
                kw["accum_out"] = accum
            S.op("act", lambda e: e.activation(out=out, in_=in_, func=func, **kw), r, w)

        def TTo(eng, out, in0, in1, op, r, w):
            S.op(eng, lambda e: e.tensor_tensor(out=out, in0=in0, in1=in1, op=op), r, w)

        def TS(eng, out, in0, s1, op0, r, w, s2=None, op1=None):
            if op1 is None:
                S.op(eng, lambda e: e.tensor_scalar(out=out, in0=in0, scalar1=s1, scalar2=None, op0=op0), r, w)
            else:
                S.op(eng, lambda e: e.tensor_scalar(out=out, in0=in0, scalar1=s1, scalar2=s2, op0=op0, op1=op1), r, w)

        def STT(eng, out, in0, scalar, in1, op0, op1, r, w, accum=None):
            if accum is None:
                S.op(eng, lambda e: e.scalar_tensor_tensor(out=out, in0=in0, scalar=scalar, in1=in1, op0=op0, op1=op1), r, w)
            else:
                S.op(eng, lambda e: e.scalar_tensor_tensor(out=out, in0=in0, scalar=scalar, in1=in1, op0=op0, op1=op1,
                                                           accum_out=accum), r, w)

        def CP(eng, out, in_, r, w):
            if eng == "act":
                S.op("act", lambda e: e.copy(out=out, in_=in_), r, w)
            else:
                S.op(eng, lambda e: e.tensor_copy(out=out, in_=in_), r, w)

        def f2(ap):
            return ap.rearrange("p a b -> p (a b)")

        def load_w(wname, c0):
            bi, KC, cw = WB[(wname, c0)]
            nb_ = NWB + len(wb_extra) if prefix_mode[0] else NWB
            i = wsel[0] % nb_
            wsel[0] = (i + 1) % nb_
            flat = (wbt[i][:, 0:KC * cw] if i < NWB else wb_extra[i - NWB][:, 0:KC * cw])
            view = flat.rearrange("p (k c) -> p k c", k=KC)
            src = wscr[bi][:, 0:KC * cw]
            key = "wb%d" % i
            S.dma("sp", lambda e: e.dma_start(out=flat, in_=src), key, reads=["wscr%d" % wgrp[bi]], writes=[key])
            return view, key, KC, cw

        pj = [0]

        def proj_bank():
            pj[0] ^= 1
            return pj[0]

        def rstd_from(out, in_, n, r, w):
            ACT(out, in_, AF.Ln, r, w, bias=EPS, scale=1.0 / n)
            ACT(out, out, AF.Exp, w, w, scale=-0.5)

        def norm_fm(srcT, srckey, lnw, dstT, dstkey, f32copy=False):
            ACT(f2(sq[:]), f2(srcT), AF.Square, [srckey], ["sq"])
            b = 2
            for k in range(8):
                MM(ps[b][:, 0:128], ones[:], sq[:, k, :], k == 0, k == 7, ["ones", "sq"], [PK[b]])
            rstd_from(rstd_bc[:], ps[b][:, 0:128], 1024.0, [PK[b]], ["rstd_bc"])
            for k in range(8):
                STT("dve", dstT[:, k, :], srcT[:, k, :], lnw[:, k:k + 1], rstd_bc[:], ALU.mult, ALU.mult,
                    [srckey, "rstd_bc", "smallA"], [dstkey])
            if f32copy:
                for k in range(8):
                    STT("dve", sq[:, k, :], srcT[:, k, :], lnw[:, k:k + 1], rstd_bc[:], ALU.mult, ALU.mult,
                        [srckey, "rstd_bc", "smallA"], ["sq"])

        def fm_proj(wname, c0, ncols, actT, akey, evac):
            o = 0
            while o < ncols:
                wv, wk, KC, cw = load_w(wname, c0 + o)
                b = proj_bank()
                sw = min(cw, 128)
                for sub in range(max(1, cw // 128)):
                    for k in range(KC):
                        MM(ps[b][0:sw, sub * 128:(sub + 1) * 128], wv[:, k, sub * sw:(sub + 1) * sw], actT[:, k, :],
                           k == 0, k == KC - 1, [wk, akey], [PK[b]])
                evac(b, o, cw)
                o += cw

        def tm_proj(wname, c0, ncols, evac):
            o = 0
            while o < ncols:
                wv, wk, KC, cw = load_w(wname, c0 + o)
                b = proj_bank()
                for k in range(8):
                    MM(ps[b][:, 0:cw], hnT[:, k, :], wv[:, k, :], k == 0, k == 7, [wk, "hnT"], [PK[b]])
                evac(b, o, cw)
                o += cw

        xev = [0]

        def mixer_tile(xsrc, ti, full, mask_ap, xi, last_prefix=False):
            xt = xT[xi]; xkey = "xT%d" % xi
            srcv = xsrc.rearrange("(k p) t -> p k t", p=128)[:, :, ti * 128:(ti + 1) * 128]
            S.dma("sp", lambda e: e.dma_start(out=xt[:], in_=srcv), xkey, writes=[xkey])
            norm_fm(xt[:], xkey, lnmix, hnT, "hnT")
            mkeys = ["maskp"] if mask_ap is not None else []
            PL = "dve"
            if full:
                fm_proj("w_in", C_Q, 512, hnT, "hnT",
                        lambda b, o, cw: S.op("act", lambda e: e.mul(out=f2(qT[:, o // 128:o // 128 + cw // 128, :]), in_=ps[b][:, 0:cw], mul=128.0 ** -0.5),
                                              [PK[b]], ["qT"]))
                fm_proj("w_in", C_K, 512, hnT, "hnT", lambda b, o, cw: CP("act", f2(kT[:, o // 128:o // 128 + cw // 128, :]), ps[b][:, 0:cw], [PK[b]], ["kT"]))
            if mask_ap is None:
                tm_proj("w_in", C_K, 512, lambda b, o, cw: CP("dve", k_tok[:, o:o + cw], ps[b][:, 0:cw], [PK[b]], ["k_tok"]))
            else:
                tm_proj("w_in", C_K, 512, lambda b, o, cw: TS("dve", k_tok[:, o:o + cw], ps[b][:, 0:cw], mask_ap, ALU.mult, [PK[b]] + mkeys, ["k_tok"]))
            tm_proj("w_in", C_V, 1024, lambda b, o, cw: CP("act", v_tok[:, o:o + cw], ps[b][:, 0:cw], [PK[b]], ["v_tok"]))
            fm_proj("w_in", C_GL, 16, hnT, "hnT", lambda b, o, cw: CP("dve", glaug[0:16, :], ps[b][0:16, 0:128], [PK[b]], ["glaug"]))
            b = 3
            MM(ps[b][:, :], glaug[0:17, :], w2aug[:, :], True, True, ["glaug", "w2aug"], [PK[b]])
            ACT(tmp512[:], ps[b][:, :], AF.Exp, [PK[b]], ["tmp512"], scale=-1.0)
            if mask_ap is None:
                ACT(lgp[:], tmp512[:], AF.Ln, ["tmp512"], ["lgp"], bias=1.0)
            else:
                ACT(tmp512[:], tmp512[:], AF.Ln, ["tmp512"], ["tmp512"], bias=1.0)
                TS("dve", lgp[:], tmp512[:], mask_ap, ALU.mult, ["tmp512"] + mkeys, ["lgp"])
            def ev_xbc(b, o, cw):
                c0_ = o // 128
                n_ = cw // 128
                j = 0
                while j < n_:
                    s0 = XPERM.index(c0_ + j)
                    L = 1
                    while j + L < n_ and XPERM.index(c0_ + j + L) == s0 + L:
                        L += 1
                    xev[0] ^= 1
                    CP("act" if xev[0] else "dve", xbc[:, s0:s0 + L, 3:131],
                       ps[b][:, j * 128:(j + L) * 128].rearrange("p (a t) -> p a t", a=L), [PK[b]], ["xbc"])
                    j += L
            fm_proj("w_in", C_XBC, 3072 if (full or last_prefix) else 2560, hnT, "hnT", ev_xbc)

            def ev_dt(b, o, cw):
                TTo("dve", dt_sb[:], ps[b][:, 0:32], dtb_bc, ALU.add, [PK[b], "smallB"], ["dt_sb"])
                ACT(dt_sb[:], dt_sb[:], AF.Exp, ["dt_sb"], ["dt_sb"])
                ACT(dt_sb[:], dt_sb[:], AF.Ln, ["dt_sb"], ["dt_sb"], bias=1.0)
                if mask_ap is not None:
                    TS("dve", dt_sb[:], dt_sb[:], mask_ap, ALU.mult, ["dt_sb"] + mkeys, ["dt_sb"])
                TTo("dve", a_sb[:], dt_sb[:], negA[:], ALU.mult, ["dt_sb", "negA"], ["a_sb"])
            tm_proj("w_in", C_DT, 32, ev_dt)

            b = 2
            for h in range(4):
                MM(ps[b][:, h * 128:(h + 1) * 128], lgp[:, h * 128:(h + 1) * 128], CR(1), True, True, ["lgp", "consts"], [PK[b]])
            ACT(f2(eG[:]), ps[b][:, :], AF.Exp, [PK[b]], ["eG"])
            if full:
                ACT(f2(eGn[:]), ps[b][:, :], AF.Exp, [PK[b]], ["eGn"], scale=-1.0)
                TTo("dve", qe[:], qT[:], eG[:], ALU.mult, ["qT", "eG"], ["qe"])
                TTo("dve", qn[:], qT[:], eGn[:], ALU.mult, ["qT", "eGn"], ["qn"])
                TTo("dve", ke[:], kT[:], eG[:], ALU.mult, ["kT", "eG"], ["ke"])
                TTo("dve", kn[:], kT[:], eGn[:], ALU.mult, ["kT", "eGn"], ["kn"])
                TTo("dve", qe0[:], qe[:].bitcast(F32), CF(7).unsqueeze(1).to_broadcast([128, 4, 128]), ALU.mult, ["qe", "consts"], ["qe0"])
                TTo("dve", qe1[:], qe[:].bitcast(F32), CF(8).unsqueeze(1).to_broadcast([128, 4, 128]), ALU.mult, ["qe", "consts"], ["qe1"])
                for h in range(4):
                    MM(ps[4][:, h * 128:(h + 1) * 128], kn[:, h, :], qe[:, h, :], True, True, ["kn", "qe"], [PK[4]])
                    MM(ps[5][:, h * 128:(h + 1) * 128], ke[:, h, :], qn[:, h, :], True, True, ["ke", "qn"], [PK[5]])
                TTo("dve", attC[:], ps[4][:, :].rearrange("p (h t) -> p h t", h=4), CF(3).unsqueeze(1).to_broadcast([128, 4, 128]),
                    ALU.mult, [PK[4], "consts"], ["attC"])
                TTo("dve", attA[:], ps[5][:, :].rearrange("p (h t) -> p h t", h=4), CF(4).unsqueeze(1).to_broadcast([128, 4, 128]),
                    ALU.mult, [PK[5], "consts"], ["attA"])
            b = 3
            MM(ps[b][:, :], CR(2), lgp[:, :], True, True, ["lgp", "consts"], [PK[b]])
            ACT(tmp512[:], ps[b][:, :], AF.Exp, [PK[b]], ["tmp512"])
            TTo("dve", kdec[:], k_tok[:], tmp512[:], ALU.mult, ["k_tok", "tmp512"], ["kdec"])
            for h in range(4):
                sk = "Sg%d" % h
                ob = 4 + h // 2
                oap = ps[ob][:, (h % 2) * 256:(h % 2) * 256 + 256]
                vh = v_tok[:, h * 256:(h + 1) * 256]
                if full:
                    MM(oap, attC[:, h, :], vh, True, False, ["attC", "v_tok"], [PK[ob]])
                    MM(oap, attA[:, h, :], vh, False, False, ["attA", "v_tok"], [PK[ob]])
                    MM(oap, qe0[:, h, :], Sg[:, h, :], False, False, ["qe0", sk], [PK[ob]])
                for c in range(2):
                    sb_ = 0 + c
                    MM(ps[sb_][:, 0:256], kdec[c * 64:(c + 1) * 64, h * 128:(h + 1) * 128], v_tok[c * 64:(c + 1) * 64, h * 256:(h + 1) * 256],
                       True, True, ["kdec", "v_tok"], [PK[sb_]])
                    STT("dve", Sg[:, h, :], Sg[:, h, :].bitcast(F32), eG[:, h, c * 64 + 63:c * 64 + 64], ps[sb_][:, 0:256], ALU.mult, ALU.add,
                        [sk, "eG", PK[sb_]], [sk])
                    if full and c == 0:
                        MM(oap, qe1[:, h, :], Sg[:, h, :], False, True, ["qe1", sk], [PK[ob]])
            if full:
                tm_proj("w_in", C_GO, 1024, lambda b, o, cw: ACT(sg[:, o:o + cw], ps[b][:, 0:cw], AF.Silu, [PK[b]], ["sg"]))
                for h in range(4):
                    ob = 4 + h // 2
                    oap = ps[ob][:, (h % 2) * 256:(h % 2) * 256 + 256]
                    S.op("act", lambda e, oap=oap, h=h: e.activation(out=tmp512[:, 0:256], in_=oap, func=AF.Square,
                                                                    accum_out=st8[:, h:h + 1]), [PK[ob]], ["st8", "tmp512"])
                rstd_from(st8[:, 4:8], st8[:, 0:4], 256.0, ["st8"], ["st8"])
                for hh in range(2):
                    TTo("dve", o_sb[:, hh * 512:(hh + 1) * 512].rearrange("p (h v) -> p h v", h=2),
                        ps[4 + hh][:, :].rearrange("p (h v) -> p h v", h=2),
                        st8[:, 4 + 2 * hh:6 + 2 * hh].unsqueeze(2).to_broadcast([128, 2, 256]), ALU.mult, [PK[4 + hh], "st8"], ["o_sb"])
                TTo(PL, o_sb[:], o_sb[:], sg[:], ALU.mult, ["o_sb", "sg"], ["o_sb"])
                for hh in range(2):
                    b = 2 + hh
                    for j in range(4):
                        cch = hh * 4 + j
                        TR(ps[b][:, j * 128:(j + 1) * 128], o_sb[:, cch * 128:(cch + 1) * 128], ["o_sb"], [PK[b]])
                    for j in range(4):
                        cch = hh * 4 + j
                        S.op("act", lambda e, b=b, j=j, cch=cch: e.mul(out=oT[:, cch, :], in_=ps[b][:, j * 128:(j + 1) * 128], mul=gnw_fm[:, cch % 2:cch % 2 + 1]),
                             [PK[b], "smallA"], ["oT"])

            b = 3
            for j, ci in enumerate((3, 4, 5, 6)):
                MM(ps[b][:, j * 32:(j + 1) * 32], CR(ci), a_sb[:, :], True, True, ["consts", "a_sb"], [PK[b]])
            ACT(f2(ex4[:]), ps[b][:, 0:128], AF.Exp, [PK[b]], ["ex4"])
            for hp in range(2):
                hs = slice(16 * hp, 16 * hp + 16)
                xw = xbc[:, 12 * hp:12 * hp + 12, :]
                for i in range(4):
                    win = xw[:, :, i:i + 128]
                    wbc = convw[:, 12 * hp:12 * hp + 12, i:i + 1].to_broadcast([128, 12, 128])
                    if i == 0:
                        TTo("dve", acc[:], win, wbc, ALU.mult, ["xbc", "smallA"], ["acc"])
                    else:
                        eng = PL if i % 2 else "dve"
                        TTo(eng, acc2[:], win, wbc, ALU.mult, ["xbc", "smallA"], ["acc2"])
                        TTo(eng, acc[:], acc[:], acc2[:], ALU.add, ["acc", "acc2"], ["acc"])
                TTo("dve", acc[:], acc[:], convb[:, 12 * hp:12 * hp + 12].unsqueeze(2).to_broadcast([128, 12, 128]), ALU.add, ["acc", "smallA"], ["acc"])
                if hp == 1:
                    CP(PL, xbc[:, :, 0:3], xbc[:, :, 128:131], ["xbc"], ["xbc"])
                ACT(f2(xsT[:]), f2(acc[:, 0:8, :]), AF.Silu, ["acc"], ["xsT"])
                ACT(f2(bcT[:]), f2(acc[:, 8:12, :]), AF.Silu, ["acc"], ["bcT"])
                for q2 in range(2):
                    b = 4 + q2
                    for j in range(4):
                        TR(ps[b][:, j * 128:(j + 1) * 128], xsT[:, q2 * 4 + j, :], ["xsT"], [PK[b]])
                    TTo("dve", xdt[:, q2 * 512:(q2 + 1) * 512].rearrange("p (h v) -> p h v", h=8),
                        ps[b][:, :].rearrange("p (h v) -> p h v", h=8),
                        dt_sb[:, 16 * hp + q2 * 8:16 * hp + q2 * 8 + 8].unsqueeze(2).to_broadcast([128, 8, 64]), ALU.mult, [PK[b], "dt_sb"], ["xdt"])
                    if full:
                        CP("act", xs_tok[:, q2 * 512:(q2 + 1) * 512], ps[b][:, :], [PK[b]], ["xs_tok"])
                b = 2
                for j in range(2):
                    TR(ps[b][:, j * 128:(j + 1) * 128], bcT[:, j, :].bitcast(F32), ["bcT"], [PK[b]])
                CP("act", B_tok[:], ps[b][:, 0:256], [PK[b]], ["B_tok"])
                expA = ex4[:, 0, hs]; toend = ex4[:, 1, hs]
                TTo("dve", xdte[:].rearrange("p (h v) -> p h v", h=16), xdt[:].bitcast(F32).rearrange("p (h v) -> p h v", h=16),
                    toend.unsqueeze(2).to_broadcast([128, 16, 64]), ALU.mult, ["xdt", "ex4"], ["xdte"])
                if full:
                    TTo("dve", R1[:], a_sb[:, hs].bitcast(F32).unsqueeze(2).to_broadcast([128, 16, 64]),
                        triL.unsqueeze(1).to_broadcast([128, 16, 64]), ALU.mult, ["a_sb", "consts"], ["R1"])
                    CP("dve", R2[:], a_sb[:, hs].bitcast(F32).unsqueeze(2).to_broadcast([128, 16, 64]), ["a_sb"], ["R2"])
                    for gl in range(2):
                        MM(ps[3][:, gl * 128:(gl + 1) * 128], bcT[:, gl, :], bcT[:, 2 + gl, :], True, True, ["bcT"], [PK[3]])
                    for gl in range(2):
                        b = 0 + gl
                        MM(ps[b][:, :], bd_ones[:], f2(R1[:, 8 * gl:8 * gl + 8, :]), True, False, ["bd", "R1"], [PK[b]])
                        MM(ps[b][:, :], negC[:], f2(R2[:, 8 * gl:8 * gl + 8, :]), False, True, ["bd", "R2"], [PK[b]])
                        ACT(seg[:, gl * 512:(gl + 1) * 512], ps[b][:, :], AF.Abs, [PK[b]], ["seg"])
                    ACT(seg[:], seg[:], AF.Exp, ["seg"], ["seg"], scale=-1.0)
                    for c in range(2):
                        pslc = slice(c * 64, (c + 1) * 64)
                        TTo("dve", MT[pslc, :, c * 64:(c + 1) * 64].rearrange("p (g r) t -> p g r t", g=2),
                            seg[pslc, :].rearrange("p (g r t) -> p g r t", g=2, r=8),
                            ps[3][pslc, 0:256].rearrange("p (g t) -> p g t", g=2)[:, :, c * 64:(c + 1) * 64].unsqueeze(2).to_broadcast([64, 2, 8, 64]),
                            ALU.mult, ["seg", PK[3]], ["MT"])
                    TTo("dve", Cm0[:], bcT[:, 2:4, :].bitcast(F32), CF(7).unsqueeze(1).to_broadcast([128, 2, 128]), ALU.mult, ["bcT", "consts"], ["Cm0"])
                    TTo("dve", Cm1[:], bcT[:, 2:4, :].bitcast(F32), CF(8).unsqueeze(1).to_broadcast([128, 2, 128]), ALU.mult, ["bcT", "consts"], ["Cm1"])
                for gl in range(2):
                    g = 2 * hp + gl
                    sk = "ST%d" % g
                    STg = ST[:, g * 512:(g + 1) * 512]
                    yb = 2
                    if full:
                        MM(ps[yb][:, :], Cm0[:, gl, :], STg, True, False, ["Cm0", sk], [PK[yb]])
                    for c in range(2):
                        sb_ = 0 + c
                        MM(ps[sb_][:, :], B_tok[c * 64:(c + 1) * 64, gl * 128:(gl + 1) * 128], xdte[c * 64:(c + 1) * 64, gl * 512:(gl + 1) * 512],
                           True, True, ["B_tok", "xdte"], [PK[sb_]])
                        TTo("dve", STg.rearrange("p (h v) -> p h v", h=8), STg.bitcast(F32).rearrange("p (h v) -> p h v", h=8),
                            ex4[:, 2 + c, g * 8:(g + 1) * 8].unsqueeze(2).to_broadcast([128, 8, 64]), ALU.mult, [sk, "ex4"], [sk])
                        TTo("dve", STg, STg.bitcast(F32), ps[sb_][:, :], ALU.add, [sk, PK[sb_]], [sk])
                        if full and c == 0:
                            MM(ps[yb][:, :], Cm1[:, gl, :], STg, False, True, ["Cm1", sk], [PK[yb]])
                    if full:
                        TTo("dve", yi[:, gl * 512:(gl + 1) * 512].rearrange("p (h v) -> p h v", h=8),
                            ps[yb][:, :].rearrange("p (h v) -> p h v", h=8),
                            expA[:, gl * 8:(gl + 1) * 8].unsqueeze(2).to_broadcast([128, 8, 64]), ALU.mult, [PK[yb], "ex4"], ["yi"])
                if not full:
                    continue
                for hl in range(16):
                    b = 4 + hl // 8
                    MM(ps[b][:, (hl % 8) * 64:(hl % 8) * 64 + 64], MT[:, hl, :], xdt[:, hl * 64:(hl + 1) * 64], True, True, ["MT", "xdt"], [PK[b]])
                for gl in range(2):
                    TTo("dve", y_sb[:, gl * 512:(gl + 1) * 512], ps[4 + gl][:, :], yi[:, gl * 512:(gl + 1) * 512], ALU.add, [PK[4 + gl], "yi"], ["y_sb"])
                TTo(PL, t2[:].rearrange("p (h v) -> p h v", h=16), xs_tok[:].rearrange("p (h v) -> p h v", h=16),
                    dsk_bc[:, hs].unsqueeze(2).to_broadcast([128, 16, 64]), ALU.mult, ["xs_tok", "smallB"], ["t2"])
                TTo(PL, y_sb[:], y_sb[:], t2[:], ALU.add, ["y_sb", "t2"], ["y_sb"])
                tm_proj("w_in", C_Z + hp * 1024, 1024, lambda b, o, cw: ACT(sz[:, o:o + cw], ps[b][:, 0:cw], AF.Silu, [PK[b]], ["sz"]))
                TTo("dve", y_sb[:], y_sb[:], sz[:], ALU.mult, ["y_sb", "sz"], ["y_sb"])
                for gl in range(2):
                    S.op("act", lambda e, gl=gl: e.activation(out=t2[:, 0:512], in_=y_sb[:, gl * 512:(gl + 1) * 512], func=AF.Square,
                                                             accum_out=st8[:, 8 + gl:9 + gl]), ["y_sb"], ["st8", "t2"])
                rstd_from(st8[:, 12:14], st8[:, 8:10], 512.0, ["st8"], ["st8"])
                TTo("dve", y_sb[:].rearrange("p (g v) -> p g v", g=2), y_sb[:].rearrange("p (g v) -> p g v", g=2),
                    st8[:, 12:14].unsqueeze(2).to_broadcast([128, 2, 512]), ALU.mult, ["y_sb", "st8"], ["y_sb"])
                for q2 in range(2):
                    b = 4 + q2
                    for j in range(4):
                        TR(ps[b][:, j * 128:(j + 1) * 128], y_sb[:, (q2 * 4 + j) * 128:(q2 * 4 + j + 1) * 128], ["y_sb"], [PK[b]])
                    for j in range(4):
                        cch = 8 * hp + q2 * 4 + j
                        S.op("act", lambda e, b=b, j=j, cch=cch: e.mul(out=yT[:, cch, :], in_=ps[b][:, j * 128:(j + 1) * 128], mul=snw_fm[:, cch:cch + 1]),
                             [PK[b], "smallA"], ["yT"])
            if not full:
                return
            for half in range(2):
                fm_proj("w_in", C_GA + half * 512, 512, hnT, "hnT",
                        lambda b, o, cw: ACT(f2(ga[:, o // 128:o // 128 + cw // 128, :]), ps[b][:, 0:cw], AF.Sigmoid, [PK[b]], ["ga"]))
                fm_proj("w_in", C_GB + half * 512, 512, hnT, "hnT",
                        lambda b, o, cw: ACT(f2(gb[:, o // 128:o // 128 + cw // 128, :]), ps[b][:, 0:cw], AF.Sigmoid, [PK[b]], ["gb"]))
                fm_proj("w_up_gla", half * 512, 512, oT, "oT",
                        lambda b, o, cw: TTo("dve", m1[:, o:o + cw], ps[b][:, 0:cw], f2(ga[:, o // 128:o // 128 + cw // 128, :]), ALU.mult, [PK[b], "ga"], ["m1"]))
                fm_proj("w_up_ssd", half * 512, 512, yT, "yT",
                        lambda b, o, cw: TTo("dve", m2[:, o:o + cw], ps[b][:, 0:cw], f2(gb[:, o // 128:o // 128 + cw // 128, :]), ALU.mult, [PK[b], "gb"], ["m2"]))
                TTo("dve", f2(mixT[:, half * 4:half * 4 + 4, :]), m1[:], m2[:], ALU.add, ["m1", "m2"], ["mixT"])
            fm_proj("w_out", 0, 1024, mixT, "mixT",
                    lambda b, o, cw: TTo("dve", f2(h1T[:, o // 128:o // 128 + cw // 128, :]), ps[b][:, 0:cw], f2(xt[:, o // 128:o // 128 + cw // 128, :]), ALU.add,
                                         [PK[b], xkey], ["h1T"]))

        gsel = [0]

        def gather(table, col, eidx, kei):
            i = gsel[0] % NSLOT
            gsel[0] += 1
            key = "gs%d" % i
            sl = slots[i]
            S.dma("pool", lambda e: e.indirect_dma_start(out=sl[:, :], out_offset=None, in_=table[:, :],
                                                         in_offset=bass.IndirectOffsetOnAxis(ap=eidx[:, col:col + 1], axis=0)),
                  key, reads=[kei, "uvbf"], writes=[key])
            return sl, key

        def peer_prologue(ti):
            g = ti % 2
            h1tok, hn2tok, eidx, wsm = h1toks[g], hn2toks[g], eidxs[g], wsms[g]
            kh1, khn, kei, kws = "h1tok%d" % g, "hn2tok%d" % g, "eidx%d" % g, "wsm%d" % g
            norm_fm(h1T[:], "h1T", lnffn, hnT, "hnT", f32copy=True)
            fm_proj("peer_wq", 0, 2048, hnT, "hnT",
                    lambda b, o, cw: CP("act" if (o // 512) % 2 else "dve", f2(pqT[:, o // 128:o // 128 + cw // 128, :]), ps[b][:, 0:cw], [PK[b]], ["pqT"]))
            for blk in range(4):
                b = 4 + (blk % 2)
                for j in range(4):
                    c = blk * 4 + j
                    MM(ps[b][:, j * 128:(j + 1) * 128], pqT[:, c, :], skT[:, c, :], True, True, ["pqT", "skT"], [PK[b]])
                CP("act", f2(sc[:, blk * 4:blk * 4 + 4, :]), ps[b][:, :], [PK[b]], ["sc"])
            for hh in range(2):
                b = 2 + hh
                for j in range(4):
                    TR(ps[b][:, j * 128:(j + 1) * 128], h1T[:, hh * 4 + j, :], ["h1T"], [PK[b]])
                CP("act", h1tok[:, hh * 512:(hh + 1) * 512], ps[b][:, :], [PK[b]], [kh1])
            for hh in range(2):
                b = 2 + hh
                for j in range(4):
                    TR(ps[b][:, j * 128:(j + 1) * 128], sq[:, hh * 4 + j, :], ["sq"], [PK[b]])
                CP("act", hn2tok[:, hh * 512:(hh + 1) * 512], ps[b][:, :], [PK[b]], [khn])
            for c in range(16):
                S.op("dve", lambda e, c=c: e.max(out=stv[:, c, 0:8], in_=sc[:, c, :]), ["sc"], ["stv"])
                S.op("dve", lambda e, c=c: e.match_replace(out=work[:, 0:128], in_to_replace=stv[:, c, 0:8], in_values=sc[:, c, :], imm_value=-1e30),
                     ["sc", "stv"], ["work"])
                S.op("dve", lambda e, c=c: e.max(out=stv[:, c, 8:16], in_=work[:, 0:128]), ["work"], ["stv"])
                S.op("dve", lambda e, c=c: e.max_index(out=sti[:, c, 0:8], in_max=stv[:, c, 0:8], in_values=sc[:, c, :]), ["sc", "stv"], ["sti"])
                S.op("dve", lambda e, c=c: e.max_index(out=sti[:, c, 8:16], in_max=stv[:, c, 8:16], in_values=sc[:, c, :]), ["sc", "stv"], ["sti"])
            CP("dve", sif[:], sti[:], ["sti"], ["sif"])
            stv4 = stv.rearrange("p (h i) k -> p h i k", i=2)
            sif4 = sif.rearrange("p (h i) k -> p h i k", i=2)
            TTo("dve", cand.rearrange("p h (a b) -> p h a b", a=16),
                stv4[:, :, 0, :].unsqueeze(3).to_broadcast([128, 8, 16, 16]),
                stv4[:, :, 1, :].unsqueeze(2).to_broadcast([128, 8, 16, 16]), ALU.add, ["stv"], ["cand"])
            for h in range(8):
                S.op("dve", lambda e, h=h: e.max(out=best[:, h, 0:8], in_=cand[:, h, :]), ["cand"], ["best"])
                S.op("dve", lambda e, h=h: e.match_replace(out=work[:, :], in_to_replace=best[:, h, 0:8], in_values=cand[:, h, :], imm_value=-1e30),
                     ["cand", "best"], ["work"])
                S.op("dve", lambda e, h=h: e.max(out=best[:, h, 8:16], in_=work[:, :]), ["work"], ["best"])
                S.op("dve", lambda e, h=h: e.max_index(out=pos[:, h, 0:8], in_max=best[:, h, 0:8], in_values=cand[:, h, :]), ["cand", "best"], ["pos"])
                S.op("dve", lambda e, h=h: e.max_index(out=pos[:, h, 8:16], in_max=best[:, h, 8:16], in_values=cand[:, h, :]), ["cand", "best"], ["pos"])
            S.op("dve", lambda e: e.tensor_single_scalar(out=posa[:], in_=pos[:], scalar=4, op=ALU.logical_shift_right), ["pos"], ["posa"])
            S.op("dve", lambda e: e.tensor_single_scalar(out=posb[:], in_=pos[:], scalar=15, op=ALU.bitwise_and), ["pos"], ["posb"])
            CP("dve", af[:], posa[:], ["posa"], ["af"])
            CP("dve", bf[:], posb[:], ["posb"], ["bf"])
            io4 = iota16.unsqueeze(1).unsqueeze(1).to_broadcast([128, 8, 16, 16])
            for (srcf, half, dst, dk_) in ((af, 0, i0s, "i0s"), (bf, 1, i1s, "i1s")):
                TTo("dve", oh[:], srcf.unsqueeze(3).to_broadcast([128, 8, 16, 16]), io4, ALU.is_equal, ["af", "bf", "consts"], ["oh"])
                TTo("dve", oh[:], oh[:], sif4[:, :, half, :].unsqueeze(2).to_broadcast([128, 8, 16, 16]), ALU.mult, ["oh", "sif"], ["oh"])
                S.op("dve", lambda e, dst=dst: e.reduce_sum(out=dst, in_=oh, axis=AX.X), ["oh"], [dk_])
            STT("dve", eidx_f[:], f2(i0s), 128.0, f2(i1s), ALU.mult, ALU.add, ["i0s", "i1s"], ["eidx_f"])
            CP("dve", eidx[:], eidx_f[:], ["eidx_f"], [kei])
            TTo("dve", wsm[:], best[:], best[:, :, 0:1].to_broadcast([128, 8, 16]), ALU.subtract, ["best"], [kws])
            ACT(f2(wsm), f2(wsm), AF.Exp, [kws], [kws])
            S.op("dve", lambda e: e.reduce_sum(out=ssum, in_=wsm, axis=AX.X), [kws], ["ssum"])
            S.op("dve", lambda e: e.reciprocal(out=ssum, in_=ssum), ["ssum"], ["ssum"])
            TTo("dve", wsm[:], wsm[:], ssum.unsqueeze(2).to_broadcast([128, 8, 16]), ALU.mult, [kws, "ssum"], [kws])

        def peer_gather(ti):
            g = ti % 2
            h1tok, hn2tok, eidx, wsm = h1toks[g], hn2toks[g], eidxs[g], wsms[g]
            kh1, khn, kei, kws = "h1tok%d" % g, "hn2tok%d" % g, "eidx%d" % g, "wsm%d" % g
            S.op("dve", lambda e: e.memset(pre, 0.0), [], ["pre"])
            for j in range(128):
                sl, key = gather(u_bf, j, eidx, kei)
                STT("dve", sl[:], sl[:], 1.0, hn2tok[:], ALU.mult, ALU.mult, [key, khn], ["pre", key], accum=pre[:, j:j + 1])
                yield
            TTo("dve", gtmp[:], pre[:], pre[:], ALU.mult, ["pre"], ["gtmp"])
            TS("dve", gtmp[:], gtmp[:], 0.044715, ALU.mult, ["gtmp"], ["gtmp"], s2=1.0, op1=ALU.add)
            TTo("dve", gtmp[:], gtmp[:], pre[:], ALU.mult, ["gtmp", "pre"], ["gtmp"])
            ACT(gtmp[:], gtmp[:], AF.Sigmoid, ["gtmp"], ["gtmp"], scale=1.5957691216057308)
            TTo("dve", gtmp[:], gtmp[:], pre[:], ALU.mult, ["gtmp", "pre"], ["gtmp"])
            TTo("dve", coef[:], gtmp[:], f2(wsm), ALU.mult, ["gtmp", kws], ["coef"])
            yield
            for j in range(128):
                sl, key = gather(v_bf, j, eidx, kei)
                dg = dgs[j % NDG]; dk_ = "dg%d" % (j % NDG)
                TS("dve", dg[:], ident, coef[:, j:j + 1], ALU.mult, ["coef", "consts"], [dk_])
                MM(ps[6][:, :], dg[:], sl[:, 0:512], j == 0, j == 127, [dk_, key], [PK[6]])
                MM(ps[7][:, :], dg[:], sl[:, 512:1024], j == 0, j == 127, [dk_, key], [PK[7]])
                yield
            TTo("dve", h1tok[:, 0:512], h1tok[:, 0:512], ps[6][:, :], ALU.add, [kh1, PK[6]], [kh1])
            TTo("dve", h1tok[:, 512:1024], h1tok[:, 512:1024], ps[7][:, :], ALU.add, [kh1, PK[7]], [kh1])
            S.op("act", lambda e: e.activation(out=hn2tok[:], in_=h1tok[:], func=AF.Square, accum_out=st9[:, 0:1]), [kh1], ["st9", khn])
            rstd_from(st9[:, 1:2], st9[:, 0:1], 1024.0, ["st9"], ["st9"])
            STT("dve", h1tok[:], h1tok[:], st9[:, 1:2], lnf_bc, ALU.mult, ALU.mult, [kh1, "st9", "smallB"], [kh1])
            S.dma("sp", lambda e: e.dma_start(out=out_d[ti * 128:(ti + 1) * 128, :], in_=h1tok[:]), "ostore%d" % g,
                  reads=[kh1], writes=["o_out%d" % g])
            yield

        S.pump = conv_tables()
        S.pump_rate = max(1, (NPT * 600) // 100)
        prefix_mode[0] = True
        for ti in range(NPT):
            mixer_tile(xpT, ti, False, maskp[:, ti:ti + 1], 0, last_prefix=(ti == NPT - 1))
        prefix_mode[0] = False
        S.drain()
        S.seal("cv1", ["wscr1"])
        S.seal("cvt", ["uvbf"])
        S.pump_rate = 4
        def main_tile(ti):
            mixer_tile(xmT, ti, True, None, 0)
            if dbg:
                dv = dbg_h1T.rearrange("(k p) t -> p k t", p=128)[:, :, ti * 128:(ti + 1) * 128]
                S.dma("sp", lambda e, dv=dv: e.dma_start(out=dv, in_=h1T[:]), "dbg", reads=["h1T"], writes=["dbg_out"])
            if dbg != "mixer":
                peer_prologue(ti)
        main_tile(0)
        for ti in range(NMT):
            if dbg != "mixer":
                S.pump = peer_gather(ti)
                S._pc = 0
                S.pump_rate = 5
            if ti + 1 < NMT:
                main_tile(ti + 1)
            S.drain()
        fin = (["o_out0", "o_out1"] if dbg != "mixer" else []) + (["dbg_out"] if dbg else [])
        S.wait_all("sp", fin)
        S.limit = limit
        build.last_sched = S
        S.emit()
    return nc


def pack_shared(inp):
    f = lambda a: np.ascontiguousarray(np.asarray(a, dtype=np.float32))
    sh = {}
    sh["w_in"] = f(inp["w_in"][0])
    sh["w2aug"] = f(np.concatenate([inp["gla_w_gate2"][0], inp["gla_b_gate"][0][None, :]], axis=0))
    sh["w_up_gla"] = f(inp["w_up_gla"][0]); sh["w_up_ssd"] = f(inp["w_up_ssd"][0]); sh["w_out"] = f(inp["w_out"][0])
    sh["peer_wq"] = f(inp["peer_w_q"][0])
    sk = np.asarray(inp["peer_sub_keys"][0], np.float32)
    sh["skT"] = f(sk.reshape(16, 128, 128).transpose(2, 0, 1).reshape(128, 16 * 128))
    sh["peer_u"] = f(inp["peer_u"][0]); sh["peer_v"] = f(inp["peer_v"][0])
    sh["consts"] = make_consts()
    fm = lambda v: np.asarray(v, np.float32).reshape(-1, 128).T
    cw = np.asarray(inp["ssd_conv_w"][0], np.float32)
    convw = cw.reshape(4, 24, 128).transpose(2, 1, 0)[:, XPERM, :].reshape(128, 96)
    convb = fm(inp["ssd_conv_b"][0])[:, XPERM]
    sh["smallA"] = f(np.concatenate([fm(inp["ln_mix_w"][0]), fm(inp["ln_ffn_w"][0]), convw, convb,
                                     fm(inp["gla_norm_w"][0]), fm(inp["ssd_norm_w"][0])], axis=1))
    bc = lambda v: np.broadcast_to(np.asarray(v, np.float32).reshape(1, -1), (128, np.asarray(v).size))
    sh["smallB"] = f(np.concatenate([bc(inp["ln_final_w"]), bc(inp["ssd_dt_bias"][0]), bc(inp["ssd_a_log"][0]), bc(inp["ssd_d"][0])], axis=1))
    return sh


def core_inputs(x_b, meta, half, NM, NP):
    chunk0 = np.concatenate([np.zeros((48, D), np.float32), meta], axis=0)
    m0 = np.concatenate([np.zeros(48, np.float32), np.ones(16, np.float32)])
    if half == 0:
        pre = np.concatenate([np.zeros((NP - 64, D), np.float32), chunk0], axis=0)
        mk = np.concatenate([np.zeros(NP - 64, np.float32), m0])
        main = x_b[0:NM]
    else:
        pre = np.concatenate([np.zeros((64, D), np.float32), chunk0, x_b[0:NM]], axis=0)
        mk = np.concatenate([np.zeros(64, np.float32), m0, np.ones(NM, np.float32)])
        main = x_b[NM:2 * NM]
    assert pre.shape[0] == NP
    return {"xpT": np.ascontiguousarray(pre.T), "xmT": np.ascontiguousarray(main.T),
            "maskp": np.ascontiguousarray(mk.reshape(NP // 128, 128).T)}


_NC_CACHE = {}


def kernel(**inputs):
    x = np.asarray(inputs["x"], np.float32)
    B, L, _ = x.shape
    NM = L // 2
    NMT = NM // 128
    NPT = NMT + 1
    meta = np.asarray(inputs["meta_tokens"], np.float32)
    sh = pack_shared(inputs)
    in_maps = []
    for b in range(B):
        for half in range(2):
            m = dict(sh)
            m.update(core_inputs(x[b], meta, half, NM, NPT * 128))
            in_maps.append(m)
    key = (NPT, NMT)
    if key not in _NC_CACHE:
        _NC_CACHE[key] = build(NPT, NMT)
    nc = _NC_CACHE[key]
    res = run_bass_kernel_spmd(nc, in_maps, core_ids=list(range(len(in_maps))))
    out = np.zeros((B, L, D), np.float32)
    for b in range(B):
        for half in range(2):
            out[b, half * NM:(half + 1) * NM] = res.results[b * 2 + half]["out"]
    return out
```
